# Optimizing a Trainium2 kernel written in Bass

```python
import jax, jax.numpy as jnp
from jax import lax
import numpy as np

D_MODEL = 1024
BATCH = 16
SEQ = 256
DEPTH = 2
DEC_BATCH = 2
DEC_SEQ = 2048
PAST_LEN = 256

GRID_W = 64
HEAD_DIM = 64
D_MIX = D_MODEL
F_GROUPS = 4
F_WIDTH = F_GROUPS * HEAD_DIM
WIN_HEADS = 6
WIN_KV_HEADS = 2
WINDOW = 128
WIN_BLOCK = 128
NA_HEADS = 6
NA_ROWS = 8
NA_COLS = 16
N_EXPERTS = 16
EC_CAPACITY = 2
D_FF_EXPERT = 2816
ROPE_BASE = 10000.0
RMS_EPS = 1e-6
Q_BLOCK = 128
NEG_INF = -1e30
ATTN_SCALE = HEAD_DIM ** -0.5
WIN_Q = WIN_HEADS * HEAD_DIM
WIN_KV = WIN_KV_HEADS * HEAD_DIM
NA_W = NA_HEADS * HEAD_DIM
N_IN = F_WIDTH + WIN_Q + 2 * WIN_KV + 3 * NA_W
SPLIT_POINTS = (F_WIDTH, F_WIDTH + WIN_Q, F_WIDTH + WIN_Q + WIN_KV, F_WIDTH + WIN_Q + 2 * WIN_KV, F_WIDTH + WIN_Q + 2 * WIN_KV + NA_W, F_WIDTH + WIN_Q + 2 * WIN_KV + 2 * NA_W)

kernel_name = 'hybrid_flow_prefix_step'


def rmsnorm(x, g):
    x32 = x.astype(jnp.float32)
    y = x32 * lax.rsqrt(jnp.mean(x32 * x32, axis=-1, keepdims=True) + RMS_EPS)
    return (y * g.astype(jnp.float32)).astype(x.dtype)


def adaln(cond, w_mod, b_mod):
    m = jax.nn.silu(cond) @ w_mod + b_mod
    return jnp.split(m, 6, axis=-1)


def modulate(h, shift, scale):
    return h * (1 + scale) + shift


def project(x, g, shift, scale, w_in):
    b, s, _ = x.shape
    h = modulate(rmsnorm(x, g), shift, scale)
    f, qw, kw, vw, qn, kn, vn = jnp.split(h @ w_in, SPLIT_POINTS, axis=-1)
    hd = lambda t: t.reshape(b, s, -1, HEAD_DIM)
    return f, hd(qw), hd(kw), hd(vw), hd(qn), hd(kn), hd(vn)


def fourier_mix(f):
    b, s, _ = f.shape
    z = jnp.fft.fft2(f.astype(jnp.float32).reshape(b, s, F_GROUPS, HEAD_DIM), axes=(1, 3), norm='ortho')
    return jnp.real(z).reshape(b, s, F_WIDTH).astype(f.dtype)


def axial_rope(x):
    b, s, h, d = x.shape
    half = d // 2
    nf = half // 2
    pos = jnp.arange(s)
    inv = 1.0 / (ROPE_BASE ** (jnp.arange(nf, dtype=jnp.float32) / nf))
    ang_r = (pos // GRID_W).astype(jnp.float32)[:, None] * inv
    ang_c = (pos % GRID_W).astype(jnp.float32)[:, None] * inv

    def rot(t, ang):
        cos = jnp.cos(ang)[None, :, None, :]
        sin = jnp.sin(ang)[None, :, None, :]
        t1, t2 = t[..., :nf], t[..., nf:]
        return jnp.concatenate([t1 * cos - t2 * sin, t2 * cos + t1 * sin], axis=-1)

    x32 = x.astype(jnp.float32)
    return jnp.concatenate([rot(x32[..., :half], ang_r), rot(x32[..., half:], ang_c)], axis=-1).astype(x.dtype)


def attn_softmax(s, sink):
    if sink is None:
        return jax.nn.softmax(s, axis=-1)
    col = jnp.broadcast_to(sink.astype(jnp.float32), s.shape[:-1] + (1,))
    return jax.nn.softmax(jnp.concatenate([s, col], axis=-1), axis=-1)[..., :-1]


def context_attention(q, k, v, sink):
    b, s, h, d = q.shape
    hkv = k.shape[2]
    g = h // hkv
    nq = s // Q_BLOCK
    qb = q.reshape(b, nq, Q_BLOCK, hkv, g, d).transpose(1, 0, 2, 3, 4, 5)
    sink_b = None if sink is None else sink.reshape(hkv, g, 1, 1)

    def block(qi):
        sc = jnp.einsum('bqkgd,bpkd->bkgqp', qi, k).astype(jnp.float32) * ATTN_SCALE
        p = attn_softmax(sc, sink_b).astype(v.dtype)
        return jnp.einsum('bkgqp,bpkd->bqkgd', p, v)

    o = lax.map(block, qb)
    return o.transpose(1, 0, 2, 3, 4, 5).reshape(b, s, h * d)


def window_attention(q, k, v, ck, cv, sink):
    b, s, h, d = q.shape
    hkv = k.shape[2]
    g = h // hkv
    nb = s // WIN_BLOCK
    qb = q.reshape(b, nb, WIN_BLOCK, hkv, g, d)

    def band(t):
        tp = jnp.pad(t, ((0, 0), (WIN_BLOCK, WIN_BLOCK), (0, 0), (0, 0))).reshape(b, nb + 2, WIN_BLOCK, hkv, d)
        return jnp.concatenate([tp[:, :-2], tp[:, 1:-1], tp[:, 2:]], axis=2)

    kb, vb = band(k), band(v)
    blk = jnp.arange(nb)[:, None]
    qpos = blk * WIN_BLOCK + jnp.arange(WIN_BLOCK)[None, :]
    kpos = (blk - 1) * WIN_BLOCK + jnp.arange(3 * WIN_BLOCK)[None, :]
    kp = kpos[:, None, :]
    mask = (jnp.abs(qpos[:, :, None] - kp) <= WINDOW) & (kp >= 0) & (kp < s)
    s_loc = jnp.einsum('bnqkgd,bnjkd->bnkgqj', qb, kb).astype(jnp.float32) * ATTN_SCALE
    s_loc = jnp.where(mask[None, :, None, None], s_loc, NEG_INF)
    s_ctx = jnp.einsum('bnqkgd,bpkd->bnkgqp', qb, ck).astype(jnp.float32) * ATTN_SCALE
    p = attn_softmax(jnp.concatenate([s_loc, s_ctx], axis=-1), sink.reshape(hkv, g, 1, 1)).astype(v.dtype)
    n_loc = 3 * WIN_BLOCK
    o = jnp.einsum('bnkgqj,bnjkd->bnqkgd', p[..., :n_loc], vb) + jnp.einsum('bnkgqp,bpkd->bnqkgd', p[..., n_loc:], cv)
    return o.reshape(b, s, h * d)


def neighbourhood_attention(q, k, v, ck, cv, rpb):
    b, s, h, d = q.shape
    rows = s // GRID_W
    kr = min(NA_ROWS, rows)
    r = jnp.arange(rows)
    cq = jnp.arange(GRID_W)
    rs = jnp.clip(r - kr // 2, 0, rows - kr)
    row_idx = rs[:, None] + jnp.arange(kr)[None, :]
    cs = jnp.clip(cq - NA_COLS // 2, 0, GRID_W - NA_COLS)
    col_mask = (cq[None, :] >= cs[:, None]) & (cq[None, :] < cs[:, None] + NA_COLS)
    qg = q.reshape(b, rows, GRID_W, h, d)
    k_rows = k.reshape(b, rows, GRID_W, h, d)[:, row_idx]
    v_rows = v.reshape(b, rows, GRID_W, h, d)[:, row_idx]
    s_nb = jnp.einsum('brchd,bramhd->brhcam', qg, k_rows).astype(jnp.float32) * ATTN_SCALE
    rel_r = row_idx - r[:, None] + NA_ROWS - 1
    rel_c = jnp.clip(cq[None, :] - cq[:, None] + NA_COLS - 1, 0, 2 * NA_COLS - 2)
    bias = rpb[:, rel_r[:, None, :, None], rel_c[None, :, None, :]]
    s_nb = s_nb + bias.transpose(1, 0, 2, 3, 4).astype(jnp.float32)[None]
    s_nb = jnp.where(col_mask[None, None, None, :, None, :], s_nb, NEG_INF).reshape(b, rows, h, GRID_W, kr * GRID_W)
    s_ctx = jnp.einsum('brchd,bphd->brhcp', qg, ck).astype(jnp.float32) * ATTN_SCALE
    p = attn_softmax(jnp.concatenate([s_nb, s_ctx], axis=-1), None).astype(v.dtype)
    n_loc = kr * GRID_W
    p_nb = p[..., :n_loc].reshape(b, rows, h, GRID_W, kr, GRID_W)
    o = jnp.einsum('brhcam,bramhd->brchd', p_nb, v_rows) + jnp.einsum('brhcp,bphd->brchd', p[..., n_loc:], cv)
    return o.reshape(b, s, h * d)


def expert_choice_ffn(h, w_router, w_gate, w_up, w_down):
    b, n, d = h.shape
    cap = max(1, EC_CAPACITY * n // N_EXPERTS)
    aff = jax.nn.softmax(jnp.einsum('bnd,de->bne', h, w_router).astype(jnp.float32), axis=-1)
    gate, idx = lax.top_k(jnp.swapaxes(aff, 1, 2), cap)
    xs = jax.vmap(lambda hb, ib: hb[ib])(h, idx)
    a = jnp.einsum('becd,edf->becf', xs, w_gate)
    u = jnp.einsum('becd,edf->becf', xs, w_up)
    y = jnp.einsum('becf,efd->becd', jax.nn.silu(a) * u, w_down) * gate[..., None].astype(h.dtype)
    scatter = lambda ib, yb: jnp.zeros((n, d), h.dtype).at[ib.reshape(-1)].add(yb.reshape(-1, d))
    return jax.vmap(scatter)(idx, y)


def context_layer(x, c_ctx, w_mod, b_mod, g_mix, g_ffn, w_in, w_out, sink, w_router, w_gate, w_up, w_down):
    sh1, sc1, gt1, sh2, sc2, gt2 = adaln(c_ctx, w_mod, b_mod)
    f, qw, kw, vw, qn, kn, vn = project(x, g_mix, sh1, sc1, w_in)
    mixed = jnp.concatenate([fourier_mix(f), context_attention(qw, kw, vw, sink), context_attention(qn, kn, vn, None)], axis=-1)
    x = x + gt1 * (mixed @ w_out)
    x = x + gt2 * expert_choice_ffn(modulate(rmsnorm(x, g_ffn), sh2, sc2), w_router, w_gate, w_up, w_down)
    return x, kw, vw, kn, vn


def latent_layer(x, cond, ck_w, cv_w, ck_n, cv_n, w_mod, b_mod, g_mix, g_ffn, w_in, w_out, sink, rpb, w_router, w_gate, w_up, w_down):
    sh1, sc1, gt1, sh2, sc2, gt2 = adaln(cond, w_mod, b_mod)
    f, qw, kw, vw, qn, kn, vn = project(x, g_mix, sh1, sc1, w_in)
    mixed = jnp.concatenate([
        fourier_mix(f),
        window_attention(axial_rope(qw), axial_rope(kw), vw, ck_w, cv_w, sink),
        neighbourhood_attention(qn, kn, vn, ck_n, cv_n, rpb),
    ], axis=-1)
    x = x + gt1 * (mixed @ w_out)
    x = x + gt2 * expert_choice_ffn(modulate(rmsnorm(x, g_ffn), sh2, sc2), w_router, w_gate, w_up, w_down)
    return x


def setup_inputs(seed: int = 0) -> dict:
    key = jax.random.key(seed)
    ks = jax.random.split(key, 22)
    nrm = lambda k, shape, s: s * jax.random.normal(k, shape, jnp.float32)
    D = D_MODEL
    return {
        'x_prompt': nrm(ks[0], (BATCH, SEQ, D), 1.0),
        'x_sample': nrm(ks[1], (DEC_BATCH, DEC_SEQ, D), 1.0),
        'cache_win_k': nrm(ks[2], (DEC_BATCH, DEPTH, PAST_LEN, WIN_KV_HEADS, HEAD_DIM), 1.0),
        'cache_win_v': nrm(ks[3], (DEC_BATCH, DEPTH, PAST_LEN, WIN_KV_HEADS, HEAD_DIM), 1.0),
        'cache_nat_k': nrm(ks[4], (DEC_BATCH, DEPTH, PAST_LEN, NA_HEADS, HEAD_DIM), 1.0),
        'cache_nat_v': nrm(ks[5], (DEC_BATCH, DEPTH, PAST_LEN, NA_HEADS, HEAD_DIM), 1.0),
        'c': nrm(ks[6], (DEC_BATCH, D), 1.0),
        'c_ctx': nrm(ks[7], (D,), 1.0),
        'w_mod': nrm(ks[8], (DEPTH, D, 6 * D), 0.5 * D ** -0.5),
        'b_mod': nrm(ks[9], (DEPTH, 6 * D), 0.01),
        'g_mix': 1.0 + nrm(ks[10], (DEPTH, D), 0.01),
        'g_ffn': 1.0 + nrm(ks[11], (DEPTH, D), 0.01),
        'w_in': nrm(ks[12], (DEPTH, D, N_IN), D ** -0.5),
        'w_out': nrm(ks[13], (DEPTH, D_MIX, D), D_MIX ** -0.5),
        'win_sink': nrm(ks[14], (DEPTH, WIN_HEADS), 0.5),
        'nat_rpb': nrm(ks[15], (DEPTH, NA_HEADS, 2 * NA_ROWS - 1, 2 * NA_COLS - 1), 0.02),
        'w_router': nrm(ks[16], (DEPTH, D, N_EXPERTS), D ** -0.5),
        'w_gate': nrm(ks[17], (DEPTH, N_EXPERTS, D, D_FF_EXPERT), D ** -0.5),
        'w_up': nrm(ks[18], (DEPTH, N_EXPERTS, D, D_FF_EXPERT), D ** -0.5),
        'w_down': nrm(ks[19], (DEPTH, N_EXPERTS, D_FF_EXPERT, D), D_FF_EXPERT ** -0.5),
        'g_final': 1.0 + nrm(ks[20], (D,), 0.01),
    }


def reference(x_prompt, x_sample, cache_win_k, cache_win_v, cache_nat_k, cache_nat_v, c, c_ctx, w_mod, b_mod, g_mix, g_ffn, w_in, w_out, win_sink, nat_rpb, w_router, w_gate, w_up, w_down, g_final):
    y_p = x_prompt
    y_s = x_sample
    c_lat = c[:, None, :]
    win_k, win_v, nat_k, nat_v = [], [], [], []
    for l in range(DEPTH):
        y_p, kw, vw, kn, vn = context_layer(y_p, c_ctx, w_mod[l], b_mod[l], g_mix[l], g_ffn[l], w_in[l], w_out[l], win_sink[l], w_router[l], w_gate[l], w_up[l], w_down[l])
        win_k.append(kw)
        win_v.append(vw)
        nat_k.append(kn)
        nat_v.append(vn)
        y_s = latent_layer(y_s, c_lat, cache_win_k[:, l], cache_win_v[:, l], cache_nat_k[:, l], cache_nat_v[:, l], w_mod[l], b_mod[l], g_mix[l], g_ffn[l], w_in[l], w_out[l], win_sink[l], nat_rpb[l], w_router[l], w_gate[l], w_up[l], w_down[l])
    y_prompt = rmsnorm(y_p, g_final)
    y_sample = rmsnorm(y_s, g_final)
    new_win_k = jnp.stack(win_k, axis=1)
    new_win_v = jnp.stack(win_v, axis=1)
    new_nat_k = jnp.stack(nat_k, axis=1)
    new_nat_v = jnp.stack(nat_v, axis=1)
    return (y_prompt, y_sample, new_win_k, new_win_v, new_nat_k, new_nat_v)
```

```python
import numpy as np
from contextlib import ExitStack
import ml_dtypes
import concourse.bass as bass
import concourse.mybir as mybir
from concourse.bass_utils import run_bass_kernel_spmd

F32 = mybir.dt.float32
BF16 = mybir.dt.bfloat16
ALU = mybir.AluOpType
AF = mybir.ActivationFunctionType
AX = mybir.AxisListType
NPBF = ml_dtypes.bfloat16

D = 1024
NT = 2560
DEPTH = 2
NE = 16
FF = 2816
NFC = 22
GROUPS = [(0, 256, 'p'), (256, 256, 'p'), (512, 2048, 's')]
NEG = -30000.0


class Tile:
    def __init__(s, t, key=None, sem=None, persist=False, is_dram=False):
        s.t = t
        s.persist = persist
        s.is_dram = is_dram
        s.lastw = None
        s.readers = {}
        s.dkey = key
        s.dcnt = 0

    def __getitem__(s, idx):
        return s.t[idx]


class KB:
    def __init__(s, nc, es):
        s.nc = nc
        s.es = es
        s.names = ['pe', 'act', 'dve', 'pool', 'sp']
        s.semobj = {}
        s.dpool = []
        s.dcounts = {}
        s.gkeys = []
        s.uid = 0
        s.new_block()

    def new_block(s):
        s.bes = ExitStack()
        s.cnt = {k: 0 for k in s.names}
        s.ops = {k: [] for k in s.names}
        s.waited = {k: {} for k in s.names}
        s.bkeys = {}
        for k in s.names:
            s.uid += 1
            key = '%s_%d' % (k, s.uid)
            s.semobj[key] = s.es.enter_context(s.nc.semaphore(key))
            s.bkeys[k] = key
        s.dnext = 0
        s.btiles = []

    def dsem(s, persist=False):
        if persist:
            key = 'g_%d' % len(s.gkeys)
            s.semobj[key] = s.es.enter_context(s.nc.semaphore(key))
            s.gkeys.append(key)
            s.dcounts[key] = 0
            return key
        if s.dnext >= len(s.dpool):
            key = 'd_%d' % len(s.dpool)
            s.semobj[key] = s.es.enter_context(s.nc.semaphore(key))
            s.dpool.append(key)
            s.dcounts[key] = 0
        key = s.dpool[s.dnext]
        s.dnext += 1
        return key

    def sb(s, shape, dt, name, dma=False, persist=False):
        s.uid += 1
        st = s.es if persist else s.bes
        t = st.enter_context(s.nc.sbuf_tensor('%s_%d' % (name, s.uid), shape, dt))
        tl = Tile(t, persist=persist)
        if not persist:
            s.btiles.append(tl)
        return tl

    def ps(s, dt=F32, n=512):
        s.uid += 1
        t = s.bes.enter_context(s.nc.psum_tensor('ps_%d' % s.uid, [128, n], dt))
        return Tile(t)

    def dram(s, shape, dt, name):
        t = s.nc.dram_tensor(name, shape, dt, kind="Internal")
        return Tile(t.ap(), persist=True, is_dram=True)

    def _waits(s, e, reads, writes):
        w = {}

        def need(k, v):
            if k == s.bkeys['pe'] and e == 'pe':
                return
            if w.get(k, 0) < v:
                w[k] = v
        for t in reads:
            if t.lastw is not None:
                need(*t.lastw)
        for t in writes:
            if t.lastw is not None:
                need(*t.lastw)
            for k, v in t.readers.items():
                need(k, v)
        wl = []
        for k, v in w.items():
            if s.waited[e].get(k, 0) >= v:
                continue
            s.waited[e][k] = v
            wl.append((k, v))
        return wl

    def emit(s, e, fn, reads=(), writes=()):
        wl = s._waits(e, reads, writes)
        s.cnt[e] += 1
        key = s.bkeys[e]
        v = s.cnt[e]
        s.ops[e].append((wl, fn, (key, 1)))
        for t in reads:
            if t.readers.get(key, 0) < v:
                t.readers[key] = v
        for t in writes:
            t.lastw = (key, v)
            t.readers = {}

    def dma(s, out_ap, in_ap, reads=(), writes=(), q='sp', st=None, slow=False):
        wl = s._waits(q, reads, writes)
        if st is None:
            cands = [t for t in list(writes) + list(reads) if not t.is_dram]
            st = cands[0]
        if st.dkey is None:
            st.dkey = s.dsem(st.persist)
        key = st.dkey
        s.dcounts[key] += 16
        v = s.dcounts[key]
        if slow:
            s.ops[q].append((wl, lambda e, o=out_ap, i=in_ap: e.dma_start(out=o, in_=i, allow_slow_non_contiguous=True), (key, 16)))
        else:
            s.ops[q].append((wl, lambda e, o=out_ap, i=in_ap: e.dma_start(out=o, in_=i), (key, 16)))
        for t in reads:
            if t.readers.get(key, 0) < v:
                t.readers[key] = v
        for t in writes:
            t.lastw = (key, v)
            t.readers = {}

    def run(s):
        nc = s.nc
        final = {s.bkeys[k]: s.cnt[k] for k in s.names if s.cnt[k] > 0}
        for key in s.dpool[:s.dnext] + s.gkeys:
            if s.dcounts[key] > 0:
                final[key] = s.dcounts[key]

        def mk(e):
            def body(eng):
                for wl, fn, inc in s.ops[e]:
                    for k, v in wl:
                        eng.wait_ge(s.semobj[k], v)
                    ins = fn(eng)
                    ins.then_inc(s.semobj[inc[0]], inc[1])
                for k, v in final.items():
                    eng.wait_ge(s.semobj[k], v)
            return body
        with nc.Block() as blk:
            blk.tensor(mk('pe'))
            blk.scalar(mk('act'))
            blk.vector(mk('dve'))
            blk.gpsimd(mk('pool'))
            blk.sync(mk('sp'))
        s.bes.close()
        s.new_block()


def build():
    nc = bass.Bass("TRN2", target_bir_lowering=False)
    es = ExitStack()
    kb = KB(nc, es)

    def din(name, shape, dt=F32):
        return Tile(nc.dram_tensor(name, shape, dt, kind="ExternalInput").ap(), persist=True, is_dram=True)

    def dout(name, shape, dt=F32):
        return Tile(nc.dram_tensor(name, shape, dt, kind="ExternalOutput").ap(), persist=True, is_dram=True)

    xin = din("xin", [NT, D])
    condT_d = din("condT", [128, 16])
    wmod_d = din("w_mod", [DEPTH, D, 6 * D])
    bmodT_d = din("bmodT", [128, 96])
    gmixT_d = din("gmixT", [128, 16])
    gffn_d = din("gffn_rep", [128, 2 * D])
    gfin_d = din("gfin_rep", [128, D])
    win_d = din("w_in", [DEPTH, D, 2048])
    wout_d = din("w_out", [DEPTH, D, D])
    wr_d = din("w_router", [DEPTH, D, NE])
    wg_d = din("w_gate", [DEPTH, NE, D, FF])
    wu_d = din("w_up", [DEPTH, NE, D, FF])
    wd_d = din("w_down", [DEPTH, NE, FF, D])
    sink_d = din("sink_rep", [128, 12])
    ckw_d = din("ckw", [DEPTH, 256, 128])
    cvw_d = din("cvw", [DEPTH, 256, 128])
    ckn_d = din("ckn", [DEPTH, 256, 384])
    cvn_d = din("cvn", [DEPTH, 256, 384])
    nbias_d = din("nbias", [DEPTH, 5, 6, 128, 640])
    ident_d = din("ident", [128, 128])
    prot_d = din("prot", [128, 128])
    rcos_d = din("ropecos", [128, 2048])
    rsin_d = din("ropesin", [128, 2048])
    wmask_d = din("wmask", [128, 256], BF16)
    identb_d = din("identb", [128, 128], BF16)
    dftc_d = din("dftc", [2048, 2048], BF16)
    dfts_d = din("dfts", [2048, 2048], BF16)
    dftp_d = din("dftp", [256, 512], BF16)
    cdsd_d = din("cdsd", [256, 512], BF16)
    iotar_d = din("iota_row", [128, 512])
    iotac_d = din("iota_col", [128, 4])
    oh16_d = din("onehot16", [16, 16 * 128])

    yout = dout("yout", [NT, D])
    kwout = dout("kwout", [2, DEPTH, 256, 128])
    vwout = dout("vwout", [2, DEPTH, 256, 128])
    knout = dout("knout", [2, DEPTH, 256, 384])
    vnout = dout("vnout", [2, DEPTH, 256, 384])

    DBG = False
    if DBG:
        dbgXT = dout("dbgXT", [2, NT, D])
        dbgXF = dout("dbgXF", [2, NT, D])
        dbgAFF = dout("dbgAFF", [2, 16, NT])
        dbgMIX = dout("dbgMIX", [2, 1024, NT], BF16)
        dbgR = dout("dbgR", [2, 16, 128, 20])
    X = kb.dram([NT, D], F32, "X_scr")
    H2 = kb.dram([NT, D], BF16, "H2_scr")
    QKT = kb.dram([10 * 128, NT], BF16, "QKT_scr")
    VV = kb.dram([NT, 512], BF16, "V_scr")
    GG = kb.dram([NT, 512], BF16, "GG_scr")
    AFFT = kb.dram([16, NT], F32, "AFFT_scr")
    RNKT = kb.dram([16, NT], F32, "RNKT_scr")

    modT = kb.sb([128, 192], F32, "modT", persist=True)
    ident = kb.sb([128, 128], F32, "ident", persist=True)
    identb = kb.sb([128, 128], BF16, "identb", persist=True)
    onesb = kb.sb([128, 128], BF16, "onesb", persist=True)
    onesf = kb.sb([128, 128], F32, "onesf", persist=True)
    iotar = kb.sb([128, 512], F32, "iotar", persist=True)
    iotac = kb.sb([128, 4], F32, "iotac", persist=True)
    gmixT = kb.sb([128, 16], F32, "gmixT", persist=True)
    sinkexp = kb.sb([128, 12], F32, "sinkexp", persist=True)
    gs1T = kb.sb([128, 32], F32, "gs1T", persist=True)
    epsb = kb.sb([128, 1], F32, "epsb", persist=True)

    PS = []

    def newps(ded=0):
        PS.clear()
        for i in range(8):
            PS.append(kb.ps())
        kb.psi = 0
        kb.ded = ded

    def psum():
        n = 8 - kb.ded
        p = PS[kb.ded + kb.psi % n]
        kb.psi += 1
        return p

    def mm(ps_t, out, lhsT, rhs, start, stop, reads, skip=False):
        kb.emit('pe', lambda e, o=out, l=lhsT, r=rhs, a=start, b=stop, sk=skip: e.matmul(o, l, r, start=a, stop=b, skip_group_check=sk),
                reads=reads, writes=[ps_t])

    def tr(ps_t, out, in_, idt, reads):
        kb.emit('pe', lambda e, o=out, i=in_, d=idt: e.transpose(o, i, d), reads=reads, writes=[ps_t])

    rr = {'i': 0}

    def ew():
        rr['i'] += 1
        return ('dve', 'dve')[rr['i'] % 2]

    def mod_idx(l, cg, cond):
        return (l * 48 + cg) * 2 + cond

    newps()
    kb.dma(ident[:, :], ident_d[:, :], reads=[ident_d], writes=[ident])
    kb.dma(identb[:, :], identb_d[:, :], reads=[identb_d], writes=[identb])
    kb.dma(iotar[:, :], iotar_d[:, :], reads=[iotar_d], writes=[iotar])
    kb.dma(iotac[:, :], iotac_d[:, :], reads=[iotac_d], writes=[iotac])
    kb.dma(gmixT[:, :], gmixT_d[:, :], reads=[gmixT_d], writes=[gmixT])
    kb.dma(sinkexp[:, :], sink_d[:, :], reads=[sink_d], writes=[sinkexp])
    kb.emit('dve', lambda e: e.memset(onesb[:, :], 1.0), writes=[onesb])
    kb.emit('dve', lambda e: e.memset(onesf[:, :], 1.0), writes=[onesf])
    kb.emit('dve', lambda e: e.memset(epsb[:, :], 1e-6), writes=[epsb])
    kb.emit('act', lambda e: e.activation(sinkexp[:, :], sinkexp[:, :], AF.Exp), reads=[sinkexp], writes=[sinkexp])
    condT = kb.sb([128, 16], F32, "condT")
    siluT = kb.sb([128, 16], F32, "siluT")
    bmodT = kb.sb([128, 96], F32, "bmodT")
    kb.dma(condT[:, :], condT_d[:, :], reads=[condT_d], writes=[condT])
    kb.dma(bmodT[:, :], bmodT_d[:, :], reads=[bmodT_d], writes=[bmodT])
    kb.emit('act', lambda e: e.activation(siluT[:, :], condT[:, :], AF.Silu), reads=[condT], writes=[siluT])
    silu3 = siluT.t[:, :].rearrange("p (c k) -> p c k", c=2)
    wmb = [kb.sb([128, 8, 512], F32, "wmb") for _ in range(2)]
    for l in range(DEPTH):
        for cq in range(12):
            wt = wmb[(l * 12 + cq) % 2]
            kb.dma(wt[:, :, :], wmod_d.t[l, :, cq * 512:(cq + 1) * 512].rearrange("(k p) n -> p k n", p=128),
                   reads=[wmod_d], writes=[wt])
            for j in range(4):
                cg = cq * 4 + j
                pt = psum()
                for k in range(8):
                    mm(pt, pt[:, 0:2], wt[:, k, j * 128:(j + 1) * 128], silu3[:, :, k], k == 0, k == 7, [wt, siluT])
                i0 = mod_idx(l, cg, 0)
                kb.emit('dve', lambda e, p=pt, i0=i0, c=l * 48 + cg: e.tensor_scalar(
                    modT[:, i0:i0 + 2], p[:, 0:2], bmodT[:, c:c + 1], None, op0=ALU.add),
                    reads=[pt, bmodT], writes=[modT])
    for l in range(DEPTH):
        for cond in range(2):
            for k in range(8):
                i = mod_idx(l, 8 + k, cond)
                o = (l * 2 + cond) * 8 + k
                kb.emit('dve', lambda e, i=i, o=o, g=l * 8 + k: e.tensor_scalar(
                    gs1T[:, o:o + 1], modT[:, i:i + 1], 1.0, gmixT[:, g:g + 1], op0=ALU.add, op1=ALU.mult),
                    reads=[modT, gmixT], writes=[gs1T])
    kb.run()

    def rowrep(dst, l, cgbase, cond, extra_g=None, plus1=False):
        for k in range(8):
            i = mod_idx(l, cgbase + k, cond)
            dg = dgp[k % 2]
            kb.emit('dve', lambda e, dg=dg, i=i: e.tensor_scalar(
                dg[:, :], ident[:, :], modT[:, i:i + 1], None, op0=ALU.mult), reads=[ident, modT], writes=[dg])
            pt = psum()
            mm(pt, pt[:, 0:128], onesf[:, :], dg[:, :], True, True, [onesf, dg])
            if plus1:
                kb.emit('dve', lambda e, p=pt, k=k: e.scalar_tensor_tensor(
                    dst[:, k * 128:(k + 1) * 128], p[:, 0:128], 1.0, extra_g[:, k * 128:(k + 1) * 128],
                    op0=ALU.add, op1=ALU.mult), reads=[pt, extra_g], writes=[dst])
            else:
                kb.emit('dve', lambda e, p=pt, k=k: e.tensor_copy(dst[:, k * 128:(k + 1) * 128], p[:, 0:128]),
                        reads=[pt], writes=[dst])

    def rmsnorm_chunk(xt, xn, ssq, rstd):
        kb.emit('act', lambda e: e.activation(xn[:, :], xt[:, :], AF.Square), reads=[xt], writes=[xn])
        kb.emit('dve', lambda e: e.tensor_reduce(ssq[:, 0:1], xn[:, :], axis=AX.X, op=ALU.add), reads=[xn], writes=[ssq])
        kb.emit('dve', lambda e: e.tensor_scalar(rstd[:, 0:1], ssq[:, 0:1], 1.0 / D, 1e-6, op0=ALU.mult, op1=ALU.add),
                reads=[ssq], writes=[rstd])
        kb.emit('act', lambda e: e.activation(rstd[:, 0:1], rstd[:, 0:1], AF.Sqrt), reads=[rstd], writes=[rstd])
        kb.emit('dve', lambda e: e.reciprocal(rstd[:, 0:1], rstd[:, 0:1]), reads=[rstd], writes=[rstd])
        kb.emit('dve', lambda e: e.tensor_scalar(xn[:, :], xt[:, :], rstd[:, 0:1], None, op0=ALU.mult),
                reads=[xt, rstd], writes=[xn])

    for l in range(DEPTH):
        xsrc = xin if l == 0 else X
        newps()
        dgp = [kb.sb([128, 128], F32, "dg") for _ in range(2)]
        wib = kb.sb([128, 8, 2048 + 384], BF16, "wib")
        wst = [kb.sb([128, 2048], F32, "wst") for _ in range(2)]
        for k in range(8):
            w = wst[k % 2]
            kb.dma(w[:, :], win_d.t[l, k * 128:(k + 1) * 128, :], reads=[win_d], writes=[w])
            kb.emit(ew(), lambda e, w=w, k=k: e.tensor_copy(wib[:, k, 0:2048], w[:, :]), reads=[w], writes=[wib])
            for hh in range(2):
                kb.emit(ew(), lambda e, w=w, k=k, hh=hh: e.tensor_copy(
                    wib.t[:, k, 2048:2432].rearrange("p (a b) -> p a b", b=128)[:, :, hh * 64:(hh + 1) * 64],
                    w.t[:, 256 + hh * 192:256 + hh * 192 + 192].rearrange("p (a b) -> p a b", b=64)), reads=[w], writes=[wib])
        xtp = [kb.sb([128, D], F32, "xt") for _ in range(2)]
        xnp = [kb.sb([128, D], F32, "xn") for _ in range(2)]
        ssqp = [kb.sb([128, 1], F32, "ssq") for _ in range(2)]
        rstdp = [kb.sb([128, 1], F32, "rstd") for _ in range(2)]
        hTp = [kb.sb([128, 8, 512], BF16, "hT") for _ in range(2)]
        ostp = [kb.sb([128, 512], BF16, "ost") for _ in range(3)]
        ofp = [kb.sb([128, 512], F32, "of") for _ in range(3)]
        kvfp = [kb.sb([128, 1024], F32, "kvf") for _ in range(2)]
        vbp = [kb.sb([128, 512], BF16, "vb") for _ in range(2)]
        ggp = [kb.sb([128, 512], BF16, "ggs") for _ in range(2)]
        fTp = [kb.sb([128, 2, 512], BF16, "fT") for _ in range(2)]
        rcs = [kb.sb([128, 512], F32, "rcs") for _ in range(2)]
        rsn = [kb.sb([128, 512], F32, "rsn") for _ in range(2)]
        prot = kb.sb([128, 128], F32, "prot")
        cdsd = kb.sb([128, 2, 512], BF16, "cdsd")
        kb.dma(prot[:, :], prot_d[:, :], reads=[prot_d], writes=[prot])
        kb.dma(cdsd[:, :, :], cdsd_d.t[:, :].rearrange("(k p) n -> p k n", p=128), reads=[cdsd_d], writes=[cdsd])
        oi = 0
        fm_chunks = []
        for i in range(3):
            fm_chunks.append(([(256 + 64 * i, 64), (256 + 64 * (i + 3), 64)], 0.125))
        fm_chunks.append(([(640, 128)], 1.0))
        for i in range(3):
            fm_chunks.append(([(896 + 128 * i, 128)], 0.125))
        for i in range(3):
            fm_chunks.append(([(1280 + 128 * i, 128)], 1.0))
        for ti in range(5):
            t0 = ti * 512
            cond = 0 if ti == 0 else 1
            sample = ti > 0
            hT = hTp[ti % 2]
            for c in range(4):
                ch = ti * 4 + c
                xt = xtp[ch % 2]
                xn = xnp[ch % 2]
                kb.dma(xt[:, :], xsrc.t[ch * 128:(ch + 1) * 128, :], reads=[xsrc], writes=[xt])
                rmsnorm_chunk(xt, xn, ssqp[ch % 2], rstdp[ch % 2])
                for k in range(8):
                    pt = psum()
                    tr(pt, pt[:, 0:128], xn[:, k * 128:(k + 1) * 128], ident[:, :], [xn, ident])
                    o = (l * 2 + cond) * 8 + k
                    i = mod_idx(l, k, cond)
                    kb.emit('act', lambda e, p=pt, hT=hT, k=k, c=c, o=o, i=i: e.activation(
                        hT[:, k, c * 128:(c + 1) * 128], p[:, 0:128], AF.Identity,
                        bias=modT[:, i:i + 1], scale=gs1T[:, o:o + 1]), reads=[pt, modT, gs1T], writes=[hT])
            if sample:
                kb.dma(rcs[ti % 2][:, :], rcos_d.t[:, t0 - 512:t0], reads=[rcos_d], writes=[rcs[ti % 2]])
                kb.dma(rsn[ti % 2][:, :], rsin_d.t[:, t0 - 512:t0], reads=[rsin_d], writes=[rsn[ti % 2]])
            for ci, (cols, scl) in enumerate(fm_chunks):
                pt = psum()
                for k in range(8):
                    if len(cols) == 2:
                        lhs = wib[:, k, 2048 + ci * 128:2048 + (ci + 1) * 128]
                    else:
                        lhs = wib[:, k, cols[0][0]:cols[0][0] + 128]
                    mm(pt, pt[:, :], lhs, hT[:, k, :], k == 0, k == 7, [wib, hT])
                ost = ostp[oi % 3]
                if sample and ci < 4:
                    of = ofp[oi % 3]
                    kb.emit('act', lambda e, p=pt, of=of, scl=scl: e.activation(of[:, :], p[:, :], AF.Copy, scale=scl),
                            reads=[pt], writes=[of])
                    p2 = psum()
                    mm(p2, p2[:, :], prot[:, :], of[:, :], True, True, [prot, of])
                    of2 = ofp[(oi + 1) % 3]
                    kb.emit('dve', lambda e, p2=p2, of2=of2, r=rsn[ti % 2]: e.tensor_tensor(of2[:, :], p2[:, :], r[:, :], op=ALU.mult),
                            reads=[p2, rsn[ti % 2]], writes=[of2])
                    kb.emit('dve', lambda e, of=of, r=rcs[ti % 2]: e.tensor_tensor(of[:, :], of[:, :], r[:, :], op=ALU.mult),
                            reads=[of, rcs[ti % 2]], writes=[of])
                    kb.emit('dve', lambda e, of=of, of2=of2, ost=ost: e.tensor_tensor(ost[:, :], of[:, :], of2[:, :], op=ALU.add),
                            reads=[of, of2], writes=[ost])
                    oi += 1
                else:
                    kb.emit('act', lambda e, p=pt, ost=ost, scl=scl: e.activation(ost[:, :], p[:, :], AF.Copy, scale=scl),
                            reads=[pt], writes=[ost])
                oi += 1
                kb.dma(QKT.t[ci * 128:(ci + 1) * 128, t0:t0 + 512], ost[:, :], reads=[ost], writes=[QKT])
            fT = fTp[ti % 2]
            for j in range(2):
                pt = psum()
                for k in range(8):
                    mm(pt, pt[:, :], wib[:, k, j * 128:(j + 1) * 128], hT[:, k, :], k == 0, k == 7, [wib, hT])
                kb.emit('act', lambda e, p=pt, fT=fT, j=j: e.activation(fT[:, j, :], p[:, :], AF.Copy), reads=[pt], writes=[fT])
            for c in range(4):
                ch = ti * 4 + c
                pt = psum()
                for j in range(2):
                    mm(pt, pt[:, :], fT[:, j, c * 128:(c + 1) * 128], cdsd[:, j, :], j == 0, j == 1, [fT, cdsd])
                gs = ggp[ch % 2]
                kb.emit('act', lambda e, p=pt, gs=gs: e.activation(gs[:, :], p[:, :], AF.Copy), reads=[pt], writes=[gs])
                kb.dma(GG.t[ch * 128:(ch + 1) * 128, :], gs[:, :], reads=[gs], writes=[GG])
                kvf = kvfp[ch % 2]
                vb = vbp[ch % 2]
                specs = [(640, 256, 0), (1280, 384, 256), (1664, 384, 640)]
                for (c0, cn, o0) in specs:
                    if sample and c0 == 1280:
                        continue
                    pt = psum()
                    for k in range(8):
                        mm(pt, pt[:, 0:cn], hT[:, k, c * 128:(c + 1) * 128], wib[:, k, c0:c0 + cn], k == 0, k == 7, [wib, hT])
                    kb.emit(('act' if c0 != 1664 else 'dve'),
                            (lambda e, p=pt, kvf=kvf, o0=o0, cn=cn: e.activation(kvf[:, o0:o0 + cn], p[:, 0:cn], AF.Copy))
                            if c0 != 1664 else
                            (lambda e, p=pt, kvf=kvf, o0=o0, cn=cn: e.tensor_copy(kvf[:, o0:o0 + cn], p[:, 0:cn])),
                            reads=[pt], writes=[kvf])
                kb.emit('act', lambda e, kvf=kvf, vb=vb: e.activation(vb[:, 0:128], kvf[:, 128:256], AF.Copy), reads=[kvf], writes=[vb])
                kb.emit('act', lambda e, kvf=kvf, vb=vb: e.activation(vb[:, 128:512], kvf[:, 640:1024], AF.Copy), reads=[kvf], writes=[vb])
                kb.dma(VV.t[ch * 128:(ch + 1) * 128, :], vb[:, :], reads=[vb], writes=[VV])
                if not sample:
                    sq = ch // 2
                    r0 = (ch % 2) * 128
                    kb.dma(kwout.t[sq, l, r0:r0 + 128, :], kvf[:, 0:128], reads=[kvf], writes=[kwout])
                    kb.dma(vwout.t[sq, l, r0:r0 + 128, :], kvf[:, 128:256], reads=[kvf], writes=[vwout])
                    kb.dma(knout.t[sq, l, r0:r0 + 128, :], kvf[:, 256:640], reads=[kvf], writes=[knout])
                    kb.dma(vnout.t[sq, l, r0:r0 + 128, :], kvf[:, 640:1024], reads=[kvf], writes=[vnout])
        kb.run()

        newps()
        dgp = [kb.sb([128, 128], F32, "dg") for _ in range(2)]
        qk = kb.sb([128, 10, 2048], BF16, "qk")
        vv = kb.sb([128, 16, 512], BF16, "vv")
        gg = kb.sb([128, 16, 512], BF16, "gg")
        mixT = kb.sb([128, 8, 2048], BF16, "mixT")
        wob = kb.sb([128, 8, D], BF16, "wob")
        xtp = [kb.sb([128, D], F32, "xt") for _ in range(2)]
        xnp = [kb.sb([128, D], F32, "xn") for _ in range(2)]
        wst = xtp
        wmask = kb.sb([128, 256], BF16, "wmask")
        kb.dma(wmask[:, :], wmask_d[:, :], reads=[wmask_d], writes=[wmask])
        for k in range(8):
            w = wst[k % 2]
            if 2 <= k <= 4:
                i = k - 2
                kb.dma(w[0:64, :], wout_d.t[l, 256 + 64 * i:256 + 64 * i + 64, :], reads=[wout_d], writes=[w])
                kb.dma(w[64:128, :], wout_d.t[l, 256 + 64 * (i + 3):256 + 64 * (i + 3) + 64, :], reads=[wout_d], writes=[w])
            else:
                kb.dma(w[:, :], wout_d.t[l, k * 128:(k + 1) * 128, :], reads=[wout_d], writes=[w])
            kb.emit(ew(), lambda e, w=w, k=k: e.tensor_copy(wob[:, k, :], w[:, :]), reads=[w], writes=[wob])
        kcT = kb.sb([128, 4, 256], BF16, "kcT")
        vc = kb.sb([128, 2, 512], BF16, "vc")
        cst = xnp
        for c in range(2):
            cs = cst[c]
            kb.dma(cs[:, 0:128], ckw_d.t[l, c * 128:(c + 1) * 128, :], reads=[ckw_d], writes=[cs])
            kb.dma(cs[:, 128:512], ckn_d.t[l, c * 128:(c + 1) * 128, :], reads=[ckn_d], writes=[cs])
            for j in range(4):
                pt = psum()
                tr(pt, pt[:, 0:128], cs[:, j * 128:(j + 1) * 128], ident[:, :], [cs, ident])
                kb.emit('act', lambda e, p=pt, j=j, c=c: e.activation(kcT[:, j, c * 128:(c + 1) * 128], p[:, 0:128], AF.Copy),
                        reads=[pt], writes=[kcT])
        cst2 = xnp
        for c in range(2):
            cs = cst2[c]
            kb.dma(cs[:, 0:128], cvw_d.t[l, c * 128:(c + 1) * 128, :], reads=[cvw_d], writes=[cs])
            kb.dma(cs[:, 128:512], cvn_d.t[l, c * 128:(c + 1) * 128, :], reads=[cvn_d], writes=[cs])
            kb.emit('dve', lambda e, cs=cs, c=c: e.tensor_copy(vc[:, c, :], cs[:, 0:512]), reads=[cs], writes=[vc])
        dftp = kb.sb([128, 2, 512], BF16, "dftp")
        kb.dma(dftp[:, :, :], dftp_d.t[:, :].rearrange("(k p) n -> p k n", p=128), reads=[dftp_d], writes=[dftp])
        dcb = [kb.sb([128, 16, 128], BF16, "dcb")] * 2
        dsb = [kb.sb([128, 16, 128], BF16, "dsb")] * 2
        pTp = [kb.sb([128, 1024], BF16, "pT") for _ in range(3)]
        recp = [kb.sb([128, 128], F32, "rec") for _ in range(3)]
        nbp = [kb.sb([128, 640], F32, "nbf") for _ in range(2)]
        nbb = [kb.sb([128, 640], BF16, "nbb") for _ in range(2)]
        gt1r = kb.sb([128, D], F32, "gt1r")
        sh2r = kb.sb([128, D], F32, "sh2r")
        gs2r = kb.sb([128, D], F32, "gs2r")
        gffn = kb.sb([128, D], F32, "gffn")
        kb.dma(gffn[:, :], gffn_d.t[:, l * D:(l + 1) * D], reads=[gffn_d], writes=[gffn])
        wrt = kb.sb([128, 8, NE], F32, "wrt")
        kb.dma(wrt[:, :, :], wr_d.t[l, :, :].rearrange("(k p) n -> p k n", p=128), reads=[wr_d], writes=[wrt])
        h2p = [kb.sb([128, D], F32, "h2f")] * 2
        h2bp = [kb.sb([128, D], BF16, "h2b") for _ in range(2)]
        h2Tp = [kb.sb([128, 8, 128], F32, "h2T")] * 2
        ssqp = [kb.sb([128, 1], F32, "ssq") for _ in range(2)]
        rstdp = [kb.sb([128, 1], F32, "rstd") for _ in range(2)]
        lgp = [kb.sb([16, 128], F32, "lg") for _ in range(2)]
        sm1 = [kb.sb([16, 128], F32, "sm1") for _ in range(2)]
        affp = [kb.sb([16, 128], F32, "affc") for _ in range(2)]
        ai = {'i': 0}

        def attn(qap_fn, nq, chunks, sink_i, base, out_ap):
            n = len(chunks)
            assert n * nq <= 1024
            pts = [psum(), psum()] if n * nq > 512 else [psum()]
            per = 512 // nq
            for ci, (kT, kr, vp, vr, bias) in enumerate(chunks):
                pt = pts[ci // per]
                o = (ci % per) * nq
                mm(pt, pt[:, o:o + nq], kT, qap_fn(), True, bias is None, kr + [qk])
                if bias is not None:
                    mm(pt, pt[:, o:o + nq], identb[:, :], bias[1], False, True, [identb, bias[0]])
            pT = pTp[ai['i'] % 3]
            rec = recp[ai['i'] % 3]
            ai['i'] += 1
            for pi, pt in enumerate(pts):
                w = min(n * nq - pi * 512, 512)
                kb.emit('act', lambda e, p=pt, pT=pT, pi=pi, w=w: e.activation(pT[:, pi * 512:pi * 512 + w], p[:, 0:w], AF.Exp),
                        reads=[pt], writes=[pT])
            def phaseB():
                pn = psum()
                pd = psum()
                for ci, (kT, kr, vp, vr, bias) in enumerate(chunks):
                    mm(pn, pn[:, 0:nq], vp, pT[:, ci * nq:(ci + 1) * nq], ci == 0, ci == n - 1, vr + [pT])
                for ci in range(n):
                    mm(pd, pd[:, 0:nq], onesb[:, :], pT[:, ci * nq:(ci + 1) * nq], ci == 0, ci == n - 1, [onesb, pT])
                b = base
                if sink_i is not None:
                    kb.emit('dve', lambda e: e.tensor_scalar(rec[b:b + 64, 0:nq], pd[b:b + 64, 0:nq], sinkexp[b:b + 64, sink_i:sink_i + 1],
                                                             None, op0=ALU.add), reads=[pd, sinkexp], writes=[rec])
                    kb.emit('dve', lambda e: e.reciprocal(rec[b:b + 64, 0:nq], rec[b:b + 64, 0:nq]), reads=[rec], writes=[rec])
                else:
                    kb.emit('dve', lambda e: e.reciprocal(rec[b:b + 64, 0:nq], pd[b:b + 64, 0:nq]), reads=[pd], writes=[rec])
                kb.emit('dve', lambda e: e.tensor_tensor(out_ap, pn[b:b + 64, 0:nq], rec[b:b + 64, 0:nq], op=ALU.mult),
                        reads=[pn, rec], writes=[mixT])
            prev = ai.get('pend')
            ai['pend'] = phaseB
            if prev is not None:
                prev()

        def attn_flush():
            if ai.get('pend') is not None:
                ai['pend']()
                ai['pend'] = None

        for gi, (g0, gn, kind) in enumerate(GROUPS):
            cond = 0 if kind == 'p' else 1
            nch = gn // 128
            for ci in range(10):
                kb.dma(qk[:, ci, 0:gn], QKT.t[ci * 128:(ci + 1) * 128, g0:g0 + gn], reads=[QKT], writes=[qk])
            kb.dma(vv[:, 0:nch, :], VV.t[g0:g0 + gn, :].rearrange("(c p) n -> p c n", p=128), reads=[VV], writes=[vv])
            kb.dma(gg[:, 0:nch, :], GG.t[g0:g0 + gn, :].rearrange("(c p) n -> p c n", p=128), reads=[GG], writes=[gg])
            rowrep(gt1r, l, 16, cond)
            rowrep(sh2r, l, 24, cond)
            rowrep(gs2r, l, 32, cond, extra_g=gffn, plus1=True)
            if kind == 'p':
                for j in range(2):
                    pt = psum()
                    idx = 0
                    for sc in range(2):
                        for half in range(2):
                            mm(pt, pt[:, 0:256], gg[:, sc, half * 256 + j * 128:half * 256 + (j + 1) * 128],
                               dftp[:, sc, half * 256:(half + 1) * 256], idx == 0, idx == 3, [gg, dftp])
                            idx += 1
                    kb.emit('act', lambda e, p=pt, j=j: e.activation(mixT[:, j, 0:256], p[:, 0:256], AF.Copy),
                            reads=[pt], writes=[mixT])
            else:
                for st_ in range(16):
                    dc = dcb[st_ % 2]
                    ds_ = dsb[st_ % 2]
                    kb.dma(dc[:, :, :], dftc_d.t[:, st_ * 128:(st_ + 1) * 128].rearrange("(k p) n -> p k n", p=128),
                           reads=[dftc_d], writes=[dc])
                    kb.dma(ds_[:, :, :], dfts_d.t[:, st_ * 128:(st_ + 1) * 128].rearrange("(k p) n -> p k n", p=128),
                           reads=[dfts_d], writes=[ds_])
                    for j in range(2):
                        pt = psum()
                        for sc in range(16):
                            mm(pt, pt[:, 0:128], gg[:, sc, j * 128:(j + 1) * 128], dc[:, sc, :], sc == 0, False, [gg, dc])
                        for sc in range(16):
                            mm(pt, pt[:, 0:128], gg[:, sc, 256 + j * 128:256 + (j + 1) * 128], ds_[:, sc, :], False, sc == 15, [gg, ds_])
                        kb.emit('act', lambda e, p=pt, j=j, s0=st_ * 128: e.activation(mixT[:, j, s0:s0 + 128], p[:, 0:128], AF.Copy),
                                reads=[pt], writes=[mixT])
            if kind == 'p':
                for h in range(6):
                    base = 0 if h < 3 else 64
                    qc = h % 3
                    for qb in range(2):
                        chunks = []
                        for kc in range(2):
                            chunks.append((qk[base:base + 64, 3, kc * 128:(kc + 1) * 128], [qk],
                                           vv[:, kc, 0:128], [vv], None))
                        attn(lambda qc=qc, qb=qb, base=base: qk[base:base + 64, qc, qb * 128:(qb + 1) * 128], 128, chunks,
                             l * 6 + h, base, mixT[base:base + 64, 2 + qc, qb * 128:(qb + 1) * 128])
                for h in range(6):
                    base = (h % 2) * 64
                    pc = h // 2
                    for qb in range(2):
                        chunks = []
                        for kc in range(2):
                            chunks.append((qk[base:base + 64, 7 + pc, kc * 128:(kc + 1) * 128], [qk],
                                           vv[:, kc, 128 + pc * 128:128 + (pc + 1) * 128], [vv], None))
                        attn(lambda pc=pc, qb=qb, base=base: qk[base:base + 64, 4 + pc, qb * 128:(qb + 1) * 128], 128, chunks,
                             None, base, mixT[base:base + 64, 5 + pc, qb * 128:(qb + 1) * 128])
            else:
                for h in range(6):
                    base = 0 if h < 3 else 64
                    qc = h % 3
                    for qb in range(16):
                        chunks = []
                        for kc in (qb - 1, qb, qb + 1):
                            if kc < 0 or kc > 15:
                                continue
                            bias = None
                            if kc == qb - 1:
                                bias = (wmask, wmask[:, 0:128])
                            elif kc == qb + 1:
                                bias = (wmask, wmask[:, 128:256])
                            chunks.append((qk[base:base + 64, 3, kc * 128:(kc + 1) * 128], [qk],
                                           vv[:, kc, 0:128], [vv], bias))
                        for cc in range(2):
                            chunks.append((kcT[base:base + 64, 0, cc * 128:(cc + 1) * 128], [kcT],
                                           vc[:, cc, 0:128], [vc], None))
                        attn(lambda qc=qc, qb=qb, base=base: qk[base:base + 64, qc, qb * 128:(qb + 1) * 128], 128, chunks,
                             l * 6 + h, base, mixT[base:base + 64, 2 + qc, qb * 128:(qb + 1) * 128])
                bi = 0
                for jp in range(16):
                    case = {0: 0, 1: 1, 14: 3, 15: 4}.get(jp, 2)
                    R0 = min(max(2 * jp - 4, 0), 22)
                    kc0 = R0 // 2
                    for h in range(6):
                        base = (h % 2) * 64
                        pc = h // 2
                        nf = nbp[bi % 2]
                        nb = nbb[bi % 2]
                        bi += 1
                        kb.dma(nf[:, :], nbias_d.t[l, case, h, :, :], reads=[nbias_d], writes=[nf])
                        kb.emit('act', lambda e, nf=nf, nb=nb: e.activation(nb[:, :], nf[:, :], AF.Copy), reads=[nf], writes=[nb])
                        chunks = []
                        for i in range(5):
                            kc = kc0 + i
                            chunks.append((qk[base:base + 64, 7 + pc, kc * 128:(kc + 1) * 128], [qk],
                                           vv[:, kc, 128 + pc * 128:128 + (pc + 1) * 128], [vv], (nb, nb[:, i * 128:(i + 1) * 128])))
                        for cc in range(2):
                            chunks.append((kcT[base:base + 64, 1 + pc, cc * 128:(cc + 1) * 128], [kcT],
                                           vc[:, cc, 128 + pc * 128:128 + (pc + 1) * 128], [vc], None))
                        attn(lambda pc=pc, jp=jp, base=base: qk[base:base + 64, 4 + pc, jp * 128:(jp + 1) * 128], 128, chunks,
                             None, base, mixT[base:base + 64, 5 + pc, jp * 128:(jp + 1) * 128])
            attn_flush()
            if DBG:
                for k in range(8):
                    kb.dma(dbgMIX.t[l, k * 128:(k + 1) * 128, g0:g0 + gn], mixT[:, k, 0:gn], reads=[mixT], writes=[dbgMIX])
            for c in range(nch):
                ch = g0 // 128 + c
                xt = xtp[ch % 2]
                xn = xnp[ch % 2]
                kb.dma(xt[:, :], xsrc.t[ch * 128:(ch + 1) * 128, :], reads=[xsrc], writes=[xt])
                for hf in range(2):
                    pt = psum()
                    for k in range(8):
                        mm(pt, pt[:, :], mixT[:, k, c * 128:(c + 1) * 128], wob[:, k, hf * 512:(hf + 1) * 512], k == 0, k == 7, [mixT, wob])
                    kb.emit('dve', lambda e, p=pt, xn=xn, hf=hf: e.tensor_tensor(xn[:, hf * 512:(hf + 1) * 512], p[:, :],
                                                                                   gt1r[:, hf * 512:(hf + 1) * 512], op=ALU.mult),
                            reads=[pt, gt1r], writes=[xn])
                kb.emit('dve', lambda e, xt=xt, xn=xn: e.tensor_tensor(xt[:, :], xt[:, :], xn[:, :], op=ALU.add), reads=[xt, xn], writes=[xt])
                kb.dma(X.t[ch * 128:(ch + 1) * 128, :], xt[:, :], reads=[xt], writes=[X])
                if DBG:
                    kb.dma(dbgXT.t[l, ch * 128:(ch + 1) * 128, :], xt[:, :], reads=[xt], writes=[dbgXT])
                rmsnorm_chunk(xt, xn, ssqp[ch % 2], rstdp[ch % 2])
                h2 = h2p[ch % 2]
                h2b = h2bp[ch % 2]
                kb.emit('dve', lambda e, xn=xn, h2=h2: e.tensor_tensor(h2[:, :], xn[:, :], gs2r[:, :], op=ALU.mult), reads=[xn, gs2r], writes=[h2])
                kb.emit('dve', lambda e, h2=h2: e.tensor_tensor(h2[:, :], h2[:, :], sh2r[:, :], op=ALU.add), reads=[h2, sh2r], writes=[h2])
                kb.emit('act', lambda e, h2=h2, h2b=h2b: e.activation(h2b[:, :], h2[:, :], AF.Copy), reads=[h2], writes=[h2b])
                kb.dma(H2.t[ch * 128:(ch + 1) * 128, :], h2b[:, :], reads=[h2b], writes=[H2])
                h2T = h2Tp[ch % 2]
                for k in range(8):
                    pt = psum()
                    tr(pt, pt[:, 0:128], h2[:, k * 128:(k + 1) * 128], ident[:, :], [h2, ident])
                    kb.emit('act' if k % 2 else 'dve',
                            (lambda e, p=pt, h2T=h2T, k=k: e.activation(h2T[:, k, :], p[:, 0:128], AF.Copy)) if k % 2 else
                            (lambda e, p=pt, h2T=h2T, k=k: e.tensor_copy(h2T[:, k, :], p[:, 0:128])),
                            reads=[pt], writes=[h2T])
                pt = psum()
                for k in range(8):
                    mm(pt, pt[0:16, 0:128], wrt[:, k, :], h2T[:, k, :], k == 0, k == 7, [wrt, h2T])
                lg = lgp[ch % 2]
                kb.emit('act', lambda e, p=pt, lg=lg: e.activation(lg[:, :], p[0:16, 0:128], AF.Exp), reads=[pt], writes=[lg])
                p2 = psum()
                mm(p2, p2[0:16, 0:128], onesf[0:16, 0:16], lg[:, :], True, True, [onesf, lg])
                s1 = sm1[ch % 2]
                kb.emit('dve', lambda e, p2=p2, s1=s1: e.reciprocal(s1[:, :], p2[0:16, 0:128]), reads=[p2], writes=[s1])
                af = affp[ch % 2]
                kb.emit('dve', lambda e, lg=lg, s1=s1, af=af: e.tensor_tensor(af[:, :], lg[:, :], s1[:, :], op=ALU.mult),
                        reads=[lg, s1], writes=[af])
                kb.dma(AFFT.t[:, ch * 128:(ch + 1) * 128], af[:, :], reads=[af], writes=[AFFT])
                if DBG:
                    kb.dma(dbgAFF.t[l, :, ch * 128:(ch + 1) * 128], af[:, :], reads=[af], writes=[dbgAFF])
        kb.run()

        newps(ded=6)
        dgp = [kb.sb([128, 128], F32, "dg") for _ in range(2)]
        acc = kb.sb([128, 20, D], F32, "acc")
        for ch in range(20):
            kb.dma(acc[:, ch, :], X.t[ch * 128:(ch + 1) * 128, :], reads=[X], writes=[acc])
        gt2r = [kb.sb([128, D], F32, "gt2r") for _ in range(2)]
        rowrep(gt2r[0], l, 40, 0)
        rowrep(gt2r[1], l, 40, 1)
        arepL = [kb.sb([128, NT], F32, "arep")] * 2
        acolL = [kb.sb([128, 20], F32, "acol")] * 2

        def aload(ex_):
            b_ = ex_ % 2
            kb.dma(arepL[b_][:, :], AFFT.t[ex_:ex_ + 1, :].to_broadcast([128, NT]), reads=[AFFT], writes=[arepL[b_]])
            kb.dma(acolL[b_][:, :], AFFT.t[ex_, :].rearrange("(c p) -> p c", p=128), reads=[AFFT], writes=[acolL[b_]], slow=True)
        aload(0)
        rcol = kb.sb([128, 20], F32, "rcol")
        rrep = kb.sb([128, NT], F32, "rrep")
        junk = [kb.sb([128, 1024], F32, "junk") for _ in range(2)]
        nacol = kb.sb([128, 20], F32, "nacol")
        rcolA = kb.sb([128, 20], F32, "rcolA")
        rcolB = kb.sb([128, 20], F32, "rcolB")
        selp = [kb.sb([128, 256], BF16, "sel") for _ in range(3)]
        selT = kb.sb([128, 2, 2048], BF16, "selT")
        selP = kb.sb([128, 512], BF16, "selP")
        kb.emit('dve', lambda e: e.memset(selP[:, :], 0.0), writes=[selP])
        h2c = [kb.sb([128, D], BF16, "h2c") for _ in range(6)]
        xsT = kb.sb([128, 8, 320], BF16, "xsT")
        NST = 2
        wgb = [kb.sb([128, 8, 512], BF16, "wgb") for _ in range(NST)]
        wub = [kb.sb([128, 8, 512], BF16, "wub") for _ in range(NST)]
        wdb = [kb.sb([128, 4, D], BF16, "wdb") for _ in range(NST)]
        FT = [(0, 512), (512, 512), (1024, 512), (1536, 512), (2048, 512), (2560, 256)]
        NFT = len(FT)

        def wload(idx):
            ex_, ft_ = divmod(idx, NFT)
            s_ = idx % NST
            f0_, fw_ = FT[ft_]
            kb.dma(wgb[s_][:, :, 0:fw_], wg_d.t[l, ex_, :, f0_:f0_ + fw_].rearrange("(k p) n -> p k n", p=128), reads=[wg_d], writes=[wgb[s_]], q='pool')
            kb.dma(wub[s_][:, :, 0:fw_], wu_d.t[l, ex_, :, f0_:f0_ + fw_].rearrange("(k p) n -> p k n", p=128), reads=[wu_d], writes=[wub[s_]], q='pool')
            kb.dma(wdb[s_][:, 0:fw_ // 128, :], wd_d.t[l, ex_, f0_:f0_ + fw_, :].rearrange("(c p) n -> p c n", p=128), reads=[wd_d], writes=[wdb[s_]], q='pool')
        for i_ in range(NST - 1):
            wload(i_)
        sg = [kb.sb([128, 320], F32, "sg") for _ in range(2)]
        hm = [kb.sb([128, 320], BF16, "hm") for _ in range(2)]
        ysb = kb.sb([128, 3, D], BF16, "ysb")
        grp = [(512, 2048, 256, 0, 1), (0, 256, 32, 256, 0), (256, 256, 32, 288, 0)]
        jis = {'i': 0}

        def rank_prep(ex_):
            if ex_ > 0:
                aload(ex_)
            kb.emit('dve', lambda e: e.tensor_scalar(nacol[:, :], acolL[0][:, :], -1.0, None, op0=ALU.mult), reads=[acolL[0]], writes=[nacol])

        def rank_pieces():
            lst = []
            for (t0, n, cap, s0, cond) in grp:
                for c in range(n // 128):
                    ch = t0 // 128 + c
                    pieces = [(t0, 1024, rcolA), (t0 + 1024, 1024, rcolB)] if n == 2048 else [(t0, n, rcolA)]
                    for (p0, pn_, dst) in pieces:
                        def piece(ch=ch, p0=p0, pn_=pn_, dst=dst):
                            jk = junk[jis['i'] % 2]
                            jis['i'] += 1
                            kb.emit('act', lambda e, jk=jk: e.activation(
                                jk[:, 0:pn_], arepL[0][:, p0:p0 + pn_], AF.Sign, bias=nacol[:, ch:ch + 1]),
                                reads=[arepL[0], nacol], writes=[jk])
                            kb.emit('dve', lambda e, jk=jk: e.tensor_reduce(dst[:, ch:ch + 1], jk[:, 0:pn_], axis=AX.X, op=ALU.add),
                                    reads=[jk], writes=[dst])
                        lst.append(piece)
            return lst
        rank_prep(0)
        for pc_ in rank_pieces():
            pc_()
        ji = 0
        for ex in range(NE):
            arep = arepL[0]
            acol = acolL[0]
            kb.emit('dve', lambda e: e.tensor_tensor(rcol[:, 4:20], rcolA[:, 4:20], rcolB[:, 4:20], op=ALU.add), reads=[rcolA, rcolB], writes=[rcol])
            kb.emit('dve', lambda e: e.tensor_scalar(rcol[:, 4:20], rcol[:, 4:20], 0.5, 1023.5, op0=ALU.mult, op1=ALU.add), reads=[rcol], writes=[rcol])
            kb.emit('dve', lambda e: e.tensor_scalar(rcol[:, 0:4], rcolA[:, 0:4], 0.5, 127.5, op0=ALU.mult, op1=ALU.add), reads=[rcolA], writes=[rcol])
            if DBG:
                kb.dma(dbgR.t[l, ex, :, :], rcol[:, :], reads=[rcol], writes=[dbgR])
            kb.dma(RNKT.t[ex, :].rearrange("(c p) -> p c", p=128), rcol[:, :], reads=[rcol], writes=[RNKT], slow=True)
            kb.dma(rrep[:, :], RNKT.t[ex:ex + 1, :].to_broadcast([128, NT]), reads=[RNKT], writes=[rrep])
            si = 0
            for (t0, n, cap, s0, cond) in grp:
                nchg = n // 128
                pts = PS[0:4] if cap == 256 else [PS[4]]
                for c in range(nchg):
                    ch = t0 // 128 + c
                    sel = selp[si % 3]
                    hc = h2c[si % 6]
                    si += 1
                    kb.dma(hc[:, :], H2.t[ch * 128:(ch + 1) * 128, :], reads=[H2], writes=[hc])
                    kb.emit('dve', lambda e, sel=sel, ch=ch, cap=cap: e.tensor_scalar(
                        sel[:, 0:cap], iotar[:, 0:cap], rcol[:, ch:ch + 1], None, op0=ALU.is_equal),
                        reads=[iotar, rcol], writes=[sel])
                    for k in range(8):
                        if cap == 256:
                            pt = pts[k // 2]
                            o = (k % 2) * 256
                        else:
                            pt = pts[0]
                            o = k * 32
                        first = (c == 0) and ((k % 2 == 0) if cap == 256 else (k == 0))
                        mm(pt, pt[:, o:o + cap], hc[:, k * 128:(k + 1) * 128], sel[:, 0:cap], first, c == nchg - 1, [hc, sel], skip=True)
                for k in range(8):
                    if cap == 256:
                        pt = pts[k // 2]
                        o = (k % 2) * 256
                    else:
                        pt = pts[0]
                        o = k * 32
                    use_act = ((k // 2) % 2 == 1) if cap == 256 else True
                    kb.emit('act' if use_act else 'dve',
                            (lambda e, p=pt, o=o, k=k, s0=s0, cap=cap: e.activation(xsT[:, k, s0:s0 + cap], p[:, o:o + cap], AF.Copy)) if use_act else
                            (lambda e, p=pt, o=o, k=k, s0=s0, cap=cap: e.tensor_copy(xsT[:, k, s0:s0 + cap], p[:, o:o + cap])),
                            reads=[pt], writes=[xsT])
            kb.emit('dve', lambda e, arep=arep: e.scalar_tensor_tensor(selT[:, 0, :], rrep[:, 512:2560], iotac[:, 0:1], arep[:, 512:2560],
                                                            op0=ALU.is_equal, op1=ALU.mult), reads=[rrep, iotac, arep], writes=[selT])
            kb.emit('dve', lambda e, arep=arep: e.scalar_tensor_tensor(selT[:, 1, :], rrep[:, 512:2560], iotac[:, 1:2], arep[:, 512:2560],
                                                             op0=ALU.is_equal, op1=ALU.mult), reads=[rrep, iotac, arep], writes=[selT])
            kb.emit('dve', lambda e, arep=arep: e.scalar_tensor_tensor(selP[0:32, 0:256], rrep[0:32, 0:256], iotac[0:32, 0:1], arep[0:32, 0:256],
                                                            op0=ALU.is_equal, op1=ALU.mult), reads=[rrep, iotac, arep], writes=[selP])
            kb.emit('dve', lambda e, arep=arep: e.scalar_tensor_tensor(selP[32:64, 256:512], rrep[32:64, 256:512], iotac[32:64, 2:3], arep[32:64, 256:512],
                                                            op0=ALU.is_equal, op1=ALU.mult), reads=[rrep, iotac, arep], writes=[selP])
            nxt = []
            if ex + 1 < NE:
                rank_prep(ex + 1)
                nxt = rank_pieces()
            py = PS[0:4]
            ypr = PS[4:6]
            pend = None

            def down(fc_, b_, ws_, j_):
                for sc in range(2):
                    for hf in range(2):
                        pt = py[sc * 2 + hf]
                        mm(pt, pt[:, :], hm[b_][:, sc * 128:(sc + 1) * 128], wdb[ws_][:, j_, hf * 512:(hf + 1) * 512], fc_ == 0, fc_ == NFC - 1, [hm[b_], wdb[ws_]])
                for hf in range(2):
                    mm(ypr[hf], ypr[hf][0:64, :], hm[b_][:, 256:320],
                       wdb[ws_][:, j_, hf * 512:(hf + 1) * 512], fc_ == 0, fc_ == NFC - 1, [hm[b_], wdb[ws_]])
            for ft in range(NFT):
                widx = ex * NFT + ft
                ws = widx % NST
                for j in range(FT[ft][1] // 128):
                    fc = FT[ft][0] // 128 + j
                    b = fc % 2
                    pa = psum()
                    pu = psum()
                    for k in range(8):
                        mm(pa, pa[:, 0:320], wgb[ws][:, k, j * 128:(j + 1) * 128], xsT[:, k, :], k == 0, k == 7, [wgb[ws], xsT])
                    for k in range(8):
                        mm(pu, pu[:, 0:320], wub[ws][:, k, j * 128:(j + 1) * 128], xsT[:, k, :], k == 0, k == 7, [wub[ws], xsT])
                    if pend is not None:
                        down(*pend)
                        pend = None
                    if j == 0 and widx + NST - 1 < NE * NFT:
                        wload(widx + NST - 1)
                    kb.emit('act', lambda e, pa=pa, b=b: e.activation(sg[b][:, :], pa[:, 0:320], AF.Silu), reads=[pa], writes=[sg[b]])
                    kb.emit('dve', lambda e, pu=pu, b=b: e.tensor_tensor(hm[b][:, :], pu[:, 0:320], sg[b][:, :], op=ALU.mult),
                            reads=[pu, sg[b]], writes=[hm[b]])
                    for _ in range(2):
                        if nxt:
                            nxt.pop(0)()
                    pend = (fc, b, ws, j)
            down(*pend)
            while nxt:
                nxt.pop(0)()
            for sc in range(2):
                for hf in range(2):
                    pt = py[sc * 2 + hf]
                    kb.emit('dve', lambda e, p=pt, sc=sc, hf=hf: e.tensor_tensor(ysb[:, sc, hf * 512:(hf + 1) * 512], p[:, :],
                                                                                  gt2r[1][:, hf * 512:(hf + 1) * 512], op=ALU.mult),
                            reads=[pt, gt2r[1]], writes=[ysb])
            for hf in range(2):
                kb.emit('dve', lambda e, hf=hf, p=ypr[hf]: e.tensor_tensor(ysb[0:64, 2, hf * 512:(hf + 1) * 512], p[0:64, :],
                                                                             gt2r[0][0:64, hf * 512:(hf + 1) * 512], op=ALU.mult),
                        reads=[ypr[hf], gt2r[0]], writes=[ysb])
            for ch in range(20):
                sample = ch >= 4
                cond = 1 if sample else 0
                for hf in range(2):
                    pt = PS[(ch * 2 + hf) % 8]
                    if sample:
                        for sc in range(2):
                            mm(pt, pt[:, :], selT[:, sc, (ch - 4) * 128:(ch - 3) * 128], ysb[:, sc, hf * 512:(hf + 1) * 512], sc == 0, sc == 1, [selT, ysb])
                    else:
                        mm(pt, pt[:, :], selP[0:64, ch * 128:(ch + 1) * 128], ysb[0:64, 2, hf * 512:(hf + 1) * 512], True, True, [selP, ysb])
                    kb.emit('dve', lambda e, p=pt, ch=ch, hf=hf: e.tensor_tensor(acc[:, ch, hf * 512:(hf + 1) * 512], p[:, :],
                                                                                  acc[:, ch, hf * 512:(hf + 1) * 512], op=ALU.add),
                            reads=[pt, acc], writes=[acc])
        if DBG:
            for ch in range(20):
                kb.dma(dbgXF.t[l, ch * 128:(ch + 1) * 128, :], acc[:, ch, :], reads=[acc], writes=[dbgXF])
        if l < DEPTH - 1:
            for ch in range(20):
                kb.dma(X.t[ch * 128:(ch + 1) * 128, :], acc[:, ch, :], reads=[acc], writes=[X])
        else:
            gfin = gt2r[0]
            kb.dma(gfin[:, :], gfin_d[:, :], reads=[gfin_d], writes=[gfin])
            xnp = [arepL[0], rrep]
            ssqp = [kb.sb([128, 1], F32, "ssq") for _ in range(2)]
            rstdp = [kb.sb([128, 1], F32, "rstd") for _ in range(2)]
            for ch in range(20):
                xn = xnp[ch % 2]
                ssq = ssqp[ch % 2]
                rstd = rstdp[ch % 2]
                kb.emit('act', lambda e, xn=xn, ch=ch: e.activation(xn[:, 0:D], acc[:, ch, :], AF.Square), reads=[acc], writes=[xn])
                kb.emit('dve', lambda e, xn=xn, ssq=ssq: e.tensor_reduce(ssq[:, 0:1], xn[:, 0:D], axis=AX.X, op=ALU.add), reads=[xn], writes=[ssq])
                kb.emit('dve', lambda e, ssq=ssq, rstd=rstd: e.tensor_scalar(rstd[:, 0:1], ssq[:, 0:1], 1.0 / D, 1e-6, op0=ALU.mult, op1=ALU.add),
                        reads=[ssq], writes=[rstd])
                kb.emit('act', lambda e, rstd=rstd: e.activation(rstd[:, 0:1], rstd[:, 0:1], AF.Sqrt), reads=[rstd], writes=[rstd])
                kb.emit('dve', lambda e, rstd=rstd: e.reciprocal(rstd[:, 0:1], rstd[:, 0:1]), reads=[rstd], writes=[rstd])
                kb.emit('dve', lambda e, xn=xn, rstd=rstd, ch=ch: e.scalar_tensor_tensor(xn[:, 0:D], acc[:, ch, :], rstd[:, 0:1], gfin[:, :],
                                                                                        op0=ALU.mult, op1=ALU.mult), reads=[acc, rstd, gfin], writes=[xn])
                kb.dma(yout.t[ch * 128:(ch + 1) * 128, :], xn[:, 0:D], reads=[xn], writes=[yout])
        kb.run()
    es.close()
    return nc


_NC = None


def _consts():
    c = {}
    c["ident"] = np.eye(128, dtype=np.float32)
    c["identb"] = np.eye(128, dtype=np.float32).astype(NPBF)
    P = np.zeros((128, 128), np.float32)
    for m in range(128):
        if m % 32 < 16:
            P[m, m + 16] = -1.0
        else:
            P[m, m - 16] = 1.0
    c["prot"] = np.ascontiguousarray(P.T)
    pos = np.arange(2048)
    inv = 1.0 / (10000.0 ** (np.arange(16, dtype=np.float32) / 16))
    dd = np.arange(128) % 64
    half = dd // 32
    j = dd % 16
    coord = np.where(half[:, None] == 0, (pos // 64)[None, :], (pos % 64)[None, :]).astype(np.float32)
    ang = coord * inv[j][:, None].astype(np.float32)
    c["ropecos"] = np.cos(ang).astype(np.float32)
    c["ropesin"] = np.sin(ang).astype(np.float32)
    kk = np.arange(128)[:, None]
    qq = np.arange(128)[None, :]
    prev = np.where(qq <= kk, 0.0, NEG)
    nxt = np.where(kk <= qq, 0.0, NEG)
    c["wmask"] = np.concatenate([prev, nxt], axis=1).astype(np.float32).astype(NPBF)
    s2 = np.arange(2048, dtype=np.float64)
    a = 2 * np.pi * np.outer(s2, s2) / 2048.0
    sc = 1.0 / np.sqrt(2048.0 * 64.0)
    c["dftc"] = (np.cos(a) * sc).astype(np.float32).astype(NPBF)
    c["dfts"] = (-np.sin(a) * sc).astype(np.float32).astype(NPBF)
    s1 = np.arange(256, dtype=np.float64)
    a = 2 * np.pi * np.outer(s1, s1) / 256.0
    sc = 1.0 / np.sqrt(256.0 * 64.0)
    c["dftp"] = np.concatenate([np.cos(a) * sc, -np.sin(a) * sc], axis=1).astype(np.float32).astype(NPBF)
    cc = np.arange(256)
    same = (cc[:, None] // 64) == (cc[None, :] // 64)
    b = 2 * np.pi * np.outer(cc % 64, cc % 64) / 64.0
    c["cdsd"] = np.concatenate([np.where(same, np.cos(b), 0.0), np.where(same, np.sin(b), 0.0)], axis=1).astype(np.float32).astype(NPBF)
    c["iota_row"] = np.tile(np.arange(512, dtype=np.float32)[None, :], (128, 1))
    p = np.arange(128, dtype=np.float32)
    c["iota_col"] = np.stack([p, p + 128, p - 32, p], axis=1).astype(np.float32)
    oh = np.zeros((16, 16, 128), np.float32)
    for e in range(16):
        oh[e, e, :] = 1.0
    c["onehot16"] = oh.reshape(16, 16 * 128)
    return c


def _nbias(rpb):
    out = np.full((DEPTH, 5, 6, 128, 5, 128), NEG, np.float32)
    for case, jp in enumerate([0, 1, 5, 14, 15]):
        R0 = min(max(2 * jp - 4, 0), 22)
        q = 128 * jp + np.arange(128)
        rq = q // 64
        cq = q % 64
        rs = np.clip(rq - 4, 0, 24)
        cs = np.clip(cq - 8, 0, 48)
        for i in range(5):
            k = 128 * (R0 // 2 + i) + np.arange(128)
            rk = k // 64
            ck = k % 64
            ok = (rk[:, None] >= rs[None, :]) & (rk[:, None] < rs[None, :] + 8) & \
                 (ck[:, None] >= cs[None, :]) & (ck[:, None] < cs[None, :] + 16)
            rr = np.clip(rk[:, None] - rq[None, :] + 7, 0, 14)
            rc = np.clip(ck[:, None] - cq[None, :] + 15, 0, 30)
            vals = rpb[:, :, rr, rc]
            out[:, case, :, :, i, :] = np.where(ok[None, None], vals, NEG)
    return out.reshape(DEPTH, 5, 6, 128, 640)


def kernel(x_prompt, x_sample, cache_win_k, cache_win_v, cache_nat_k, cache_nat_v, c, c_ctx, w_mod, b_mod,
           g_mix, g_ffn, w_in, w_out, win_sink, nat_rpb, w_router, w_gate, w_up, w_down, g_final):
    global _NC
    f = lambda a: np.ascontiguousarray(np.asarray(a, dtype=np.float32))
    x_prompt, x_sample = f(x_prompt), f(x_sample)
    if _NC is None:
        _NC = build()
    nc = _NC
    cst = _consts()
    shared = dict(cst)
    shared["w_mod"] = f(w_mod)
    shared["bmodT"] = f(np.asarray(b_mod).reshape(2, 48, 128).transpose(2, 0, 1).reshape(128, 96))
    shared["gmixT"] = f(np.asarray(g_mix).reshape(2, 8, 128).transpose(2, 0, 1).reshape(128, 16))
    shared["gffn_rep"] = f(np.tile(np.asarray(g_ffn).reshape(1, 2 * D), (128, 1)))
    shared["gfin_rep"] = f(np.tile(np.asarray(g_final).reshape(1, D), (128, 1)))
    shared["w_in"] = f(w_in)
    shared["w_out"] = f(w_out)
    shared["w_router"] = f(w_router)
    shared["w_gate"] = f(w_gate)
    shared["w_up"] = f(w_up)
    shared["w_down"] = f(w_down)
    shared["sink_rep"] = f(np.tile(np.asarray(win_sink).reshape(1, 12), (128, 1)))
    shared["nbias"] = _nbias(np.asarray(nat_rpb, dtype=np.float32))
    in_maps = []
    for core in range(8):
        sq = core // 4
        m = dict(shared)
        m["xin"] = np.ascontiguousarray(np.concatenate([x_prompt[2 * core], x_prompt[2 * core + 1], x_sample[sq]], axis=0))
        cond = np.stack([np.asarray(c_ctx, dtype=np.float32), np.asarray(c, dtype=np.float32)[sq]], axis=0)
        m["condT"] = f(cond.reshape(2, 8, 128).transpose(2, 0, 1).reshape(128, 16))
        m["ckw"] = f(np.asarray(cache_win_k)[sq].reshape(2, 256, 128))
        m["cvw"] = f(np.asarray(cache_win_v)[sq].reshape(2, 256, 128))
        m["ckn"] = f(np.asarray(cache_nat_k)[sq].reshape(2, 256, 384))
        m["cvn"] = f(np.asarray(cache_nat_v)[sq].reshape(2, 256, 384))
        in_maps.append(m)
    res = run_bass_kernel_spmd(nc, in_maps, core_ids=list(range(8)))
    r = res.results
    y_prompt = np.zeros((16, 256, D), np.float32)
    y_sample = np.zeros((2, 2048, D), np.float32)
    nwk = np.zeros((16, 2, 256, 2, 64), np.float32)
    nwv = np.zeros((16, 2, 256, 2, 64), np.float32)
    nnk = np.zeros((16, 2, 256, 6, 64), np.float32)
    nnv = np.zeros((16, 2, 256, 6, 64), np.float32)
    for core in range(8):
        yo = np.asarray(r[core]["yout"])
        y_prompt[2 * core] = yo[0:256]
        y_prompt[2 * core + 1] = yo[256:512]
        q = core % 4
        y_sample[core // 4, q * 512:(q + 1) * 512] = yo[512 + q * 512:512 + (q + 1) * 512]
        for j in range(2):
            nwk[2 * core + j] = np.asarray(r[core]["kwout"])[j].reshape(2, 256, 2, 64)
            nwv[2 * core + j] = np.asarray(r[core]["vwout"])[j].reshape(2, 256, 2, 64)
            nnk[2 * core + j] = np.asarray(r[core]["knout"])[j].reshape(2, 256, 6, 64)
            nnv[2 * core + j] = np.asarray(r[core]["vnout"])[j].reshape(2, 256, 6, 64)
    global _DBG
    _DBG = r
    return (y_prompt, y_sample, nwk, nwv, nnk, nnv)
```

```python
import numpy as np
from contextlib import ExitStack
import ml_dtypes
import concourse.bass as bass
import concourse.mybir as mybir
from concourse.bass_utils import run_bass_kernel_spmd

F32 = mybir.dt.float32
BF16 = mybir.dt.bfloat16
ALU = mybir.AluOpType
AF = mybir.ActivationFunctionType
AX = mybir.AxisListType
NPBF = ml_dtypes.bfloat16

D = 1024
NT = 2560
DEPTH = 2
NE = 16
FF = 2816
NFC = 22
GROUPS = [(0, 256, 'p'), (256, 256, 'p'), (512, 2048, 's')]
NEG = -30000.0


class Tile:
    def __init__(s, t, key=None, sem=None, persist=False, is_dram=False):
        s.t = t
        s.persist = persist
        s.is_dram = is_dram
        s.lastw = None
        s.readers = {}
        s.dkey = key
        s.dcnt = 0

    def __getitem__(s, idx):
        return s.t[idx]


class KB:
    def __init__(s, nc, es):
        s.nc = nc
        s.es = es
        s.names = ['pe', 'act', 'dve', 'pool', 'sp']
        s.semobj = {}
        s.dpool = []
        s.dcounts = {}
        s.gkeys = []
        s.uid = 0
        s.new_block()

    def new_block(s):
        s.bes = ExitStack()
        s.cnt = {k: 0 for k in s.names}
        s.ops = {k: [] for k in s.names}
        s.waited = {k: {} for k in s.names}
        s.bkeys = {}
        for k in s.names:
            s.uid += 1
            key = '%s_%d' % (k, s.uid)
            s.semobj[key] = s.es.enter_context(s.nc.semaphore(key))
            s.bkeys[k] = key
        s.dnext = 0
        s.btiles = []

    def dsem(s, persist=False):
        if persist:
            key = 'g_%d' % len(s.gkeys)
            s.semobj[key] = s.es.enter_context(s.nc.semaphore(key))
            s.gkeys.append(key)
            s.dcounts[key] = 0
            return key
        if s.dnext >= len(s.dpool):
            key = 'd_%d' % len(s.dpool)
            s.semobj[key] = s.es.enter_context(s.nc.semaphore(key))
            s.dpool.append(key)
            s.dcounts[key] = 0
        key = s.dpool[s.dnext]
        s.dnext += 1
        return key

    def sb(s, shape, dt, name, dma=False, persist=False):
        s.uid += 1
        st = s.es if persist else s.bes
        t = st.enter_context(s.nc.sbuf_tensor('%s_%d' % (name, s.uid), shape, dt))
        tl = Tile(t, persist=persist)
        if not persist:
            s.btiles.append(tl)
        return tl

    def ps(s, dt=F32, n=512):
        s.uid += 1
        t = s.bes.enter_context(s.nc.psum_tensor('ps_%d' % s.uid, [128, n], dt))
        return Tile(t)

    def dram(s, shape, dt, name):
        t = s.nc.dram_tensor(name, shape, dt, kind="Internal")
        return Tile(t.ap(), persist=True, is_dram=True)

    def _waits(s, e, reads, writes):
        w = {}

        def need(k, v):
            if k == s.bkeys['pe'] and e == 'pe':
                return
            if w.get(k, 0) < v:
                w[k] = v
        for t in reads:
            if t.lastw is not None:
                need(*t.lastw)
        for t in writes:
            if t.lastw is not None:
                need(*t.lastw)
            for k, v in t.readers.items():
                need(k, v)
        wl = []
        for k, v in w.items():
            if s.waited[e].get(k, 0) >= v:
                continue
            s.waited[e][k] = v
            wl.append((k, v))
        return wl

    def emit(s, e, fn, reads=(), writes=()):
        wl = s._waits(e, reads, writes)
        s.cnt[e] += 1
        key = s.bkeys[e]
        v = s.cnt[e]
        s.ops[e].append((wl, fn, (key, 1)))
        for t in reads:
            if t.readers.get(key, 0) < v:
                t.readers[key] = v
        for t in writes:
            t.lastw = (key, v)
            t.readers = {}

    def dma(s, out_ap, in_ap, reads=(), writes=(), q='sp', st=None, slow=False):
        wl = s._waits(q, reads, writes)
        if st is None:
            cands = [t for t in list(writes) + list(reads) if not t.is_dram]
            st = cands[0]
        if st.dkey is None:
            st.dkey = s.dsem(st.persist)
        key = st.dkey
        s.dcounts[key] += 16
        v = s.dcounts[key]
        if slow:
            s.ops[q].append((wl, lambda e, o=out_ap, i=in_ap: e.dma_start(out=o, in_=i, allow_slow_non_contiguous=True), (key, 16)))
        else:
            s.ops[q].append((wl, lambda e, o=out_ap, i=in_ap: e.dma_start(out=o, in_=i), (key, 16)))
        for t in reads:
            if t.readers.get(key, 0) < v:
                t.readers[key] = v
        for t in writes:
            t.lastw = (key, v)
            t.readers = {}

    def run(s):
        nc = s.nc
        final = {s.bkeys[k]: s.cnt[k] for k in s.names if s.cnt[k] > 0}
        for key in s.dpool[:s.dnext] + s.gkeys:
            if s.dcounts[key] > 0:
                final[key] = s.dcounts[key]

        def mk(e):
            def body(eng):
                for wl, fn, inc in s.ops[e]:
                    for k, v in wl:
                        eng.wait_ge(s.semobj[k], v)
                    ins = fn(eng)
                    ins.then_inc(s.semobj[inc[0]], inc[1])
                for k, v in final.items():
                    eng.wait_ge(s.semobj[k], v)
            return body
        with nc.Block() as blk:
            blk.tensor(mk('pe'))
            blk.scalar(mk('act'))
            blk.vector(mk('dve'))
            blk.gpsimd(mk('pool'))
            blk.sync(mk('sp'))
        s.bes.close()
        s.new_block()


def build():
    nc = bass.Bass("TRN2", target_bir_lowering=False)
    es = ExitStack()
    kb = KB(nc, es)

    def din(name, shape, dt=F32):
        return Tile(nc.dram_tensor(name, shape, dt, kind="ExternalInput").ap(), persist=True, is_dram=True)

    def dout(name, shape, dt=F32):
        return Tile(nc.dram_tensor(name, shape, dt, kind="ExternalOutput").ap(), persist=True, is_dram=True)

    xin = din("xin", [NT, D])
    condT_d = din("condT", [128, 16])
    wmod_d = din("w_mod", [DEPTH, D, 6 * D])
    bmodT_d = din("bmodT", [128, 96])
    gmixT_d = din("gmixT", [128, 16])
    gffn_d = din("gffn_rep", [128, 2 * D])
    gfin_d = din("gfin_rep", [128, D])
    win_d = din("w_in", [DEPTH, D, 2048])
    wout_d = din("w_out", [DEPTH, D, D])
    wr_d = din("w_router", [DEPTH, D, NE])
    wg_d = din("w_gate", [DEPTH, NE, D, FF])
    wu_d = din("w_up", [DEPTH, NE, D, FF])
    wd_d = din("w_down", [DEPTH, NE, FF, D])
    sink_d = din("sink_rep", [128, 12])
    ckw_d = din("ckw", [DEPTH, 256, 128])
    cvw_d = din("cvw", [DEPTH, 256, 128])
    ckn_d = din("ckn", [DEPTH, 256, 384])
    cvn_d = din("cvn", [DEPTH, 256, 384])
    nbias_d = din("nbias", [DEPTH, 5, 6, 128, 640])
    ident_d = din("ident", [128, 128])
    prot_d = din("prot", [128, 128])
    rcos_d = din("ropecos", [128, 2048])
    rsin_d = din("ropesin", [128, 2048])
    wmask_d = din("wmask", [128, 256], BF16)
    identb_d = din("identb", [128, 128], BF16)
    dftc_d = din("dftc", [2048, 2048], BF16)
    dfts_d = din("dfts", [2048, 2048], BF16)
    dftp_d = din("dftp", [256, 512], BF16)
    cdsd_d = din("cdsd", [256, 512], BF16)
    iotar_d = din("iota_row", [128, 512])
    iotac_d = din("iota_col", [128, 4])
    oh16_d = din("onehot16", [16, 16 * 128])

    yout = dout("yout", [NT, D])
    kwout = dout("kwout", [2, DEPTH, 256, 128])
    vwout = dout("vwout", [2, DEPTH, 256, 128])
    knout = dout("knout", [2, DEPTH, 256, 384])
    vnout = dout("vnout", [2, DEPTH, 256, 384])

    DBG = False
    if DBG:
        dbgXT = dout("dbgXT", [2, NT, D])
        dbgXF = dout("dbgXF", [2, NT, D])
        dbgAFF = dout("dbgAFF", [2, 16, NT])
        dbgMIX = dout("dbgMIX", [2, 1024, NT], BF16)
        dbgR = dout("dbgR", [2, 16, 128, 20])
    X = kb.dram([NT, D], F32, "X_scr")
    H2 = kb.dram([NT, D], BF16, "H2_scr")
    QKT = kb.dram([10 * 128, NT], BF16, "QKT_scr")
    VV = kb.dram([NT, 512], BF16, "V_scr")
    GG = kb.dram([NT, 512], BF16, "GG_scr")
    AFFT = kb.dram([16, NT], F32, "AFFT_scr")
    RNKT = kb.dram([16, NT], F32, "RNKT_scr")

    modT = kb.sb([128, 192], F32, "modT", persist=True)
    ident = kb.sb([128, 128], F32, "ident", persist=True)
    identb = kb.sb([128, 128], BF16, "identb", persist=True)
    onesb = kb.sb([128, 128], BF16, "onesb", persist=True)
    onesf = kb.sb([128, 128], F32, "onesf", persist=True)
    iotar = kb.sb([128, 512], F32, "iotar", persist=True)
    iotac = kb.sb([128, 4], F32, "iotac", persist=True)
    gmixT = kb.sb([128, 16], F32, "gmixT", persist=True)
    sinkexp = kb.sb([128, 12], F32, "sinkexp", persist=True)
    gs1T = kb.sb([128, 32], F32, "gs1T", persist=True)
    epsb = kb.sb([128, 1], F32, "epsb", persist=True)

    PS = []

    def newps(ded=0):
        PS.clear()
        for i in range(8):
            PS.append(kb.ps())
        kb.psi = 0
        kb.ded = ded

    def psum():
        n = 8 - kb.ded
        p = PS[kb.ded + kb.psi % n]
        kb.psi += 1
        return p

    def mm(ps_t, out, lhsT, rhs, start, stop, reads, skip=False):
        kb.emit('pe', lambda e, o=out, l=lhsT, r=rhs, a=start, b=stop, sk=skip: e.matmul(o, l, r, start=a, stop=b, skip_group_check=sk),
                reads=reads, writes=[ps_t])

    def tr(ps_t, out, in_, idt, reads):
        kb.emit('pe', lambda e, o=out, i=in_, d=idt: e.transpose(o, i, d), reads=reads, writes=[ps_t])

    rr = {'i': 0}

    def ew():
        rr['i'] += 1
        return ('dve', 'dve')[rr['i'] % 2]

    def mod_idx(l, cg, cond):
        return (l * 48 + cg) * 2 + cond

    newps()
    kb.dma(ident[:, :], ident_d[:, :], reads=[ident_d], writes=[ident])
    kb.dma(identb[:, :], identb_d[:, :], reads=[identb_d], writes=[identb])
    kb.dma(iotar[:, :], iotar_d[:, :], reads=[iotar_d], writes=[iotar])
    kb.dma(iotac[:, :], iotac_d[:, :], reads=[iotac_d], writes=[iotac])
    kb.dma(gmixT[:, :], gmixT_d[:, :], reads=[gmixT_d], writes=[gmixT])
    kb.dma(sinkexp[:, :], sink_d[:, :], reads=[sink_d], writes=[sinkexp])
    kb.emit('dve', lambda e: e.memset(onesb[:, :], 1.0), writes=[onesb])
    kb.emit('dve', lambda e: e.memset(onesf[:, :], 1.0), writes=[onesf])
    kb.emit('dve', lambda e: e.memset(epsb[:, :], 1e-6), writes=[epsb])
    kb.emit('act', lambda e: e.activation(sinkexp[:, :], sinkexp[:, :], AF.Exp), reads=[sinkexp], writes=[sinkexp])
    condT = kb.sb([128, 16], F32, "condT")
    siluT = kb.sb([128, 16], F32, "siluT")
    bmodT = kb.sb([128, 96], F32, "bmodT")
    kb.dma(condT[:, :], condT_d[:, :], reads=[condT_d], writes=[condT])
    kb.dma(bmodT[:, :], bmodT_d[:, :], reads=[bmodT_d], writes=[bmodT])
    kb.emit('act', lambda e: e.activation(siluT[:, :], condT[:, :], AF.Silu), reads=[condT], writes=[siluT])
    silu3 = siluT.t[:, :].rearrange("p (c k) -> p c k", c=2)
    wmb = [kb.sb([128, 8, 512], F32, "wmb") for _ in range(2)]
    for l in range(DEPTH):
        for cq in range(12):
            wt = wmb[(l * 12 + cq) % 2]
            kb.dma(wt[:, :, :], wmod_d.t[l, :, cq * 512:(cq + 1) * 512].rearrange("(k p) n -> p k n", p=128),
                   reads=[wmod_d], writes=[wt])
            for j in range(4):
                cg = cq * 4 + j
                pt = psum()
                for k in range(8):
                    mm(pt, pt[:, 0:2], wt[:, k, j * 128:(j + 1) * 128], silu3[:, :, k], k == 0, k == 7, [wt, siluT])
                i0 = mod_idx(l, cg, 0)
                kb.emit('dve', lambda e, p=pt, i0=i0, c=l * 48 + cg: e.tensor_scalar(
                    modT[:, i0:i0 + 2], p[:, 0:2], bmodT[:, c:c + 1], None, op0=ALU.add),
                    reads=[pt, bmodT], writes=[modT])
    for l in range(DEPTH):
        for cond in range(2):
            for k in range(8):
                i = mod_idx(l, 8 + k, cond)
                o = (l * 2 + cond) * 8 + k
                kb.emit('dve', lambda e, i=i, o=o, g=l * 8 + k: e.tensor_scalar(
                    gs1T[:, o:o + 1], modT[:, i:i + 1], 1.0, gmixT[:, g:g + 1], op0=ALU.add, op1=ALU.mult),
                    reads=[modT, gmixT], writes=[gs1T])
    kb.run()

    def rowrep(dst, l, cgbase, cond, extra_g=None, plus1=False):
        for k in range(8):
            i = mod_idx(l, cgbase + k, cond)
            dg = dgp[k % 2]
            kb.emit('dve', lambda e, dg=dg, i=i: e.tensor_scalar(
                dg[:, :], ident[:, :], modT[:, i:i + 1], None, op0=ALU.mult), reads=[ident, modT], writes=[dg])
            pt = psum()
            mm(pt, pt[:, 0:128], onesf[:, :], dg[:, :], True, True, [onesf, dg])
            if plus1:
                kb.emit('dve', lambda e, p=pt, k=k: e.scalar_tensor_tensor(
                    dst[:, k * 128:(k + 1) * 128], p[:, 0:128], 1.0, extra_g[:, k * 128:(k + 1) * 128],
                    op0=ALU.add, op1=ALU.mult), reads=[pt, extra_g], writes=[dst])
            else:
                kb.emit('dve', lambda e, p=pt, k=k: e.tensor_copy(dst[:, k * 128:(k + 1) * 128], p[:, 0:128]),
                        reads=[pt], writes=[dst])

    def rmsnorm_chunk(xt, xn, ssq, rstd):
        kb.emit('act', lambda e: e.activation(xn[:, :], xt[:, :], AF.Square), reads=[xt], writes=[xn])
        kb.emit('dve', lambda e: e.tensor_reduce(ssq[:, 0:1], xn[:, :], axis=AX.X, op=ALU.add), reads=[xn], writes=[ssq])
        kb.emit('dve', lambda e: e.tensor_scalar(rstd[:, 0:1], ssq[:, 0:1], 1.0 / D, 1e-6, op0=ALU.mult, op1=ALU.add),
                reads=[ssq], writes=[rstd])
        kb.emit('act', lambda e: e.activation(rstd[:, 0:1], rstd[:, 0:1], AF.Sqrt), reads=[rstd], writes=[rstd])
        kb.emit('dve', lambda e: e.reciprocal(rstd[:, 0:1], rstd[:, 0:1]), reads=[rstd], writes=[rstd])
        kb.emit('dve', lambda e: e.tensor_scalar(xn[:, :], xt[:, :], rstd[:, 0:1], None, op0=ALU.mult),
                reads=[xt, rstd], writes=[xn])

    for l in range(DEPTH):
        xsrc = xin if l == 0 else X
        newps()
        dgp = [kb.sb([128, 128], F32, "dg") for _ in range(2)]
        wib = kb.sb([128, 8, 2048 + 384], BF16, "wib")
        wst = [kb.sb([128, 2048], F32, "wst") for _ in range(2)]
        for k in range(8):
            w = wst[k % 2]
            kb.dma(w[:, :], win_d.t[l, k * 128:(k + 1) * 128, :], reads=[win_d], writes=[w])
            kb.emit(ew(), lambda e, w=w, k=k: e.tensor_copy(wib[:, k, 0:2048], w[:, :]), reads=[w], writes=[wib])
            for hh in range(2):
                kb.emit(ew(), lambda e, w=w, k=k, hh=hh: e.tensor_copy(
                    wib.t[:, k, 2048:2432].rearrange("p (a b) -> p a b", b=128)[:, :, hh * 64:(hh + 1) * 64],
                    w.t[:, 256 + hh * 192:256 + hh * 192 + 192].rearrange("p (a b) -> p a b", b=64)), reads=[w], writes=[wib])
        xtp = [kb.sb([128, D], F32, "xt") for _ in range(2)]
        xnp = [kb.sb([128, D], F32, "xn") for _ in range(2)]
        ssqp = [kb.sb([128, 1], F32, "ssq") for _ in range(2)]
        rstdp = [kb.sb([128, 1], F32, "rstd") for _ in range(2)]
        hTp = [kb.sb([128, 8, 512], BF16, "hT") for _ in range(2)]
        ostp = [kb.sb([128, 512], BF16, "ost") for _ in range(3)]
        ofp = [kb.sb([128, 512], F32, "of") for _ in range(3)]
        kvfp = [kb.sb([128, 1024], F32, "kvf") for _ in range(2)]
        vbp = [kb.sb([128, 512], BF16, "vb") for _ in range(2)]
        ggp = [kb.sb([128, 512], BF16, "ggs") for _ in range(2)]
        fTp = [kb.sb([128, 2, 512], BF16, "fT") for _ in range(2)]
        rcs = [kb.sb([128, 512], F32, "rcs") for _ in range(2)]
        rsn = [kb.sb([128, 512], F32, "rsn") for _ in range(2)]
        prot = kb.sb([128, 128], F32, "prot")
        cdsd = kb.sb([128, 2, 512], BF16, "cdsd")
        kb.dma(prot[:, :], prot_d[:, :], reads=[prot_d], writes=[prot])
        kb.dma(cdsd[:, :, :], cdsd_d.t[:, :].rearrange("(k p) n -> p k n", p=128), reads=[cdsd_d], writes=[cdsd])
        oi = 0
        fm_chunks = []
        for i in range(3):
            fm_chunks.append(([(256 + 64 * i, 64), (256 + 64 * (i + 3), 64)], 0.125))
        fm_chunks.append(([(640, 128)], 1.0))
        for i in range(3):
            fm_chunks.append(([(896 + 128 * i, 128)], 0.125))
        for i in range(3):
            fm_chunks.append(([(1280 + 128 * i, 128)], 1.0))
        for ti in range(5):
            t0 = ti * 512
            cond = 0 if ti == 0 else 1
            sample = ti > 0
            hT = hTp[ti % 2]
            for c in range(4):
                ch = ti * 4 + c
                xt = xtp[ch % 2]
                xn = xnp[ch % 2]
                kb.dma(xt[:, :], xsrc.t[ch * 128:(ch + 1) * 128, :], reads=[xsrc], writes=[xt])
                rmsnorm_chunk(xt, xn, ssqp[ch % 2], rstdp[ch % 2])
                for k in range(8):
                    pt = psum()
                    tr(pt, pt[:, 0:128], xn[:, k * 128:(k + 1) * 128], ident[:, :], [xn, ident])
                    o = (l * 2 + cond) * 8 + k
                    i = mod_idx(l, k, cond)
                    kb.emit('act', lambda e, p=pt, hT=hT, k=k, c=c, o=o, i=i: e.activation(
                        hT[:, k, c * 128:(c + 1) * 128], p[:, 0:128], AF.Identity,
                        bias=modT[:, i:i + 1], scale=gs1T[:, o:o + 1]), reads=[pt, modT, gs1T], writes=[hT])
            if sample:
                kb.dma(rcs[ti % 2][:, :], rcos_d.t[:, t0 - 512:t0], reads=[rcos_d], writes=[rcs[ti % 2]])
                kb.dma(rsn[ti % 2][:, :], rsin_d.t[:, t0 - 512:t0], reads=[rsin_d], writes=[rsn[ti % 2]])
            for ci, (cols, scl) in enumerate(fm_chunks):
                pt = psum()
                for k in range(8):
                    if len(cols) == 2:
                        lhs = wib[:, k, 2048 + ci * 128:2048 + (ci + 1) * 128]
                    else:
                        lhs = wib[:, k, cols[0][0]:cols[0][0] + 128]
                    mm(pt, pt[:, :], lhs, hT[:, k, :], k == 0, k == 7, [wib, hT])
                ost = ostp[oi % 3]
                if sample and ci < 4:
                    of = ofp[oi % 3]
                    kb.emit('act', lambda e, p=pt, of=of, scl=scl: e.activation(of[:, :], p[:, :], AF.Copy, scale=scl),
                            reads=[pt], writes=[of])
                    p2 = psum()
                    mm(p2, p2[:, :], prot[:, :], of[:, :], True, True, [prot, of])
                    of2 = ofp[(oi + 1) % 3]
                    kb.emit('dve', lambda e, p2=p2, of2=of2, r=rsn[ti % 2]: e.tensor_tensor(of2[:, :], p2[:, :], r[:, :], op=ALU.mult),
                            reads=[p2, rsn[ti % 2]], writes=[of2])
                    kb.emit('dve', lambda e, of=of, r=rcs[ti % 2]: e.tensor_tensor(of[:, :], of[:, :], r[:, :], op=ALU.mult),
                            reads=[of, rcs[ti % 2]], writes=[of])
                    kb.emit('dve', lambda e, of=of, of2=of2, ost=ost: e.tensor_tensor(ost[:, :], of[:, :], of2[:, :], op=ALU.add),
                            reads=[of, of2], writes=[ost])
                    oi += 1
                else:
                    kb.emit('act', lambda e, p=pt, ost=ost, scl=scl: e.activation(ost[:, :], p[:, :], AF.Copy, scale=scl),
                            reads=[pt], writes=[ost])
                oi += 1
                kb.dma(QKT.t[ci * 128:(ci + 1) * 128, t0:t0 + 512], ost[:, :], reads=[ost], writes=[QKT])
            fT = fTp[ti % 2]
            for j in range(2):
                pt = psum()
                for k in range(8):
                    mm(pt, pt[:, :], wib[:, k, j * 128:(j + 1) * 128], hT[:, k, :], k == 0, k == 7, [wib, hT])
                kb.emit('act', lambda e, p=pt, fT=fT, j=j: e.activation(fT[:, j, :], p[:, :], AF.Copy), reads=[pt], writes=[fT])
            for c in range(4):
                ch = ti * 4 + c
                pt = psum()
                for j in range(2):
                    mm(pt, pt[:, :], fT[:, j, c * 128:(c + 1) * 128], cdsd[:, j, :], j == 0, j == 1, [fT, cdsd])
                gs = ggp[ch % 2]
                kb.emit('act', lambda e, p=pt, gs=gs: e.activation(gs[:, :], p[:, :], AF.Copy), reads=[pt], writes=[gs])
                kb.dma(GG.t[ch * 128:(ch + 1) * 128, :], gs[:, :], reads=[gs], writes=[GG])
                kvf = kvfp[ch % 2]
                vb = vbp[ch % 2]
                specs = [(640, 256, 0), (1280, 384, 256), (1664, 384, 640)]
                for (c0, cn, o0) in specs:
                    if sample and c0 == 1280:
                        continue
                    pt = psum()
                    for k in range(8):
                        mm(pt, pt[:, 0:cn], hT[:, k, c * 128:(c + 1) * 128], wib[:, k, c0:c0 + cn], k == 0, k == 7, [wib, hT])
                    kb.emit(('act' if c0 != 1664 else 'dve'),
                            (lambda e, p=pt, kvf=kvf, o0=o0, cn=cn: e.activation(kvf[:, o0:o0 + cn], p[:, 0:cn], AF.Copy))
                            if c0 != 1664 else
                            (lambda e, p=pt, kvf=kvf, o0=o0, cn=cn: e.tensor_copy(kvf[:, o0:o0 + cn], p[:, 0:cn])),
                            reads=[pt], writes=[kvf])
                kb.emit('act', lambda e, kvf=kvf, vb=vb: e.activation(vb[:, 0:128], kvf[:, 128:256], AF.Copy), reads=[kvf], writes=[vb])
                kb.emit('act', lambda e, kvf=kvf, vb=vb: e.activation(vb[:, 128:512], kvf[:, 640:1024], AF.Copy), reads=[kvf], writes=[vb])
                kb.dma(VV.t[ch * 128:(ch + 1) * 128, :], vb[:, :], reads=[vb], writes=[VV])
                if not sample:
                    sq = ch // 2
                    r0 = (ch % 2) * 128
                    kb.dma(kwout.t[sq, l, r0:r0 + 128, :], kvf[:, 0:128], reads=[kvf], writes=[kwout])
                    kb.dma(vwout.t[sq, l, r0:r0 + 128, :], kvf[:, 128:256], reads=[kvf], writes=[vwout])
                    kb.dma(knout.t[sq, l, r0:r0 + 128, :], kvf[:, 256:640], reads=[kvf], writes=[knout])
                    kb.dma(vnout.t[sq, l, r0:r0 + 128, :], kvf[:, 640:1024], reads=[kvf], writes=[vnout])
        kb.run()

        newps()
        dgp = [kb.sb([128, 128], F32, "dg") for _ in range(2)]
        qk = kb.sb([128, 10, 2048], BF16, "qk")
        vv = kb.sb([128, 16, 512], BF16, "vv")
        gg = kb.sb([128, 16, 512], BF16, "gg")
        mixT = kb.sb([128, 8, 2048], BF16, "mixT")
        wob = kb.sb([128, 8, D], BF16, "wob")
        xtp = [kb.sb([128, D], F32, "xt") for _ in range(2)]
        xnp = [kb.sb([128, D], F32, "xn") for _ in range(2)]
        wst = xtp
        wmask = kb.sb([128, 256], BF16, "wmask")
        kb.dma(wmask[:, :], wmask_d[:, :], reads=[wmask_d], writes=[wmask])
        for k in range(8):
            w = wst[k % 2]
            if 2 <= k <= 4:
                i = k - 2
                kb.dma(w[0:64, :], wout_d.t[l, 256 + 64 * i:256 + 64 * i + 64, :], reads=[wout_d], writes=[w])
                kb.dma(w[64:128, :], wout_d.t[l, 256 + 64 * (i + 3):256 + 64 * (i + 3) + 64, :], reads=[wout_d], writes=[w])
            else:
                kb.dma(w[:, :], wout_d.t[l, k * 128:(k + 1) * 128, :], reads=[wout_d], writes=[w])
            kb.emit(ew(), lambda e, w=w, k=k: e.tensor_copy(wob[:, k, :], w[:, :]), reads=[w], writes=[wob])
        kcT = kb.sb([128, 4, 256], BF16, "kcT")
        vc = kb.sb([128, 2, 512], BF16, "vc")
        cst = xnp
        for c in range(2):
            cs = cst[c]
            kb.dma(cs[:, 0:128], ckw_d.t[l, c * 128:(c + 1) * 128, :], reads=[ckw_d], writes=[cs])
            kb.dma(cs[:, 128:512], ckn_d.t[l, c * 128:(c + 1) * 128, :], reads=[ckn_d], writes=[cs])
            for j in range(4):
                pt = psum()
                tr(pt, pt[:, 0:128], cs[:, j * 128:(j + 1) * 128], ident[:, :], [cs, ident])
                kb.emit('act', lambda e, p=pt, j=j, c=c: e.activation(kcT[:, j, c * 128:(c + 1) * 128], p[:, 0:128], AF.Copy),
                        reads=[pt], writes=[kcT])
        cst2 = xnp
        for c in range(2):
            cs = cst2[c]
            kb.dma(cs[:, 0:128], cvw_d.t[l, c * 128:(c + 1) * 128, :], reads=[cvw_d], writes=[cs])
            kb.dma(cs[:, 128:512], cvn_d.t[l, c * 128:(c + 1) * 128, :], reads=[cvn_d], writes=[cs])
            kb.emit('dve', lambda e, cs=cs, c=c: e.tensor_copy(vc[:, c, :], cs[:, 0:512]), reads=[cs], writes=[vc])
        dftp = kb.sb([128, 2, 512], BF16, "dftp")
        kb.dma(dftp[:, :, :], dftp_d.t[:, :].rearrange("(k p) n -> p k n", p=128), reads=[dftp_d], writes=[dftp])
        dcb = [kb.sb([128, 16, 128], BF16, "dcb")] * 2
        dsb = [kb.sb([128, 16, 128], BF16, "dsb")] * 2
        pTp = [kb.sb([128, 1024], BF16, "pT") for _ in range(3)]
        recp = [kb.sb([128, 128], F32, "rec") for _ in range(3)]
        nbp = [kb.sb([128, 640], F32, "nbf") for _ in range(2)]
        nbb = [kb.sb([128, 640], BF16, "nbb") for _ in range(2)]
        gt1r = kb.sb([128, D], F32, "gt1r")
        sh2r = kb.sb([128, D], F32, "sh2r")
        gs2r = kb.sb([128, D], F32, "gs2r")
        gffn = kb.sb([128, D], F32, "gffn")
        kb.dma(gffn[:, :], gffn_d.t[:, l * D:(l + 1) * D], reads=[gffn_d], writes=[gffn])
        wrt = kb.sb([128, 8, NE], F32, "wrt")
        kb.dma(wrt[:, :, :], wr_d.t[l, :, :].rearrange("(k p) n -> p k n", p=128), reads=[wr_d], writes=[wrt])
        h2p = [kb.sb([128, D], F32, "h2f")] * 2
        h2bp = [kb.sb([128, D], BF16, "h2b") for _ in range(2)]
        h2Tp = [kb.sb([128, 8, 128], F32, "h2T")] * 2
        ssqp = [kb.sb([128, 1], F32, "ssq") for _ in range(2)]
        rstdp = [kb.sb([128, 1], F32, "rstd") for _ in range(2)]
        lgp = [kb.sb([16, 128], F32, "lg") for _ in range(2)]
        sm1 = [kb.sb([16, 128], F32, "sm1") for _ in range(2)]
        affp = [kb.sb([16, 128], F32, "affc") for _ in range(2)]
        ai = {'i': 0}

        def attn(qap_fn, nq, chunks, sink_i, base, out_ap):
            n = len(chunks)
            assert n * nq <= 1024
            pts = [psum(), psum()] if n * nq > 512 else [psum()]
            per = 512 // nq
            for ci, (kT, kr, vp, vr, bias) in enumerate(chunks):
                pt = pts[ci // per]
                o = (ci % per) * nq
                mm(pt, pt[:, o:o + nq], kT, qap_fn(), True, bias is None, kr + [qk])
                if bias is not None:
                    mm(pt, pt[:, o:o + nq], identb[:, :], bias[1], False, True, [identb, bias[0]])
            pT = pTp[ai['i'] % 3]
            rec = recp[ai['i'] % 3]
            ai['i'] += 1
            for pi, pt in enumerate(pts):
                w = min(n * nq - pi * 512, 512)
                kb.emit('act', lambda e, p=pt, pT=pT, pi=pi, w=w: e.activation(pT[:, pi * 512:pi * 512 + w], p[:, 0:w], AF.Exp),
                        reads=[pt], writes=[pT])
            def phaseB():
                pn = psum()
                pd = psum()
                for ci, (kT, kr, vp, vr, bias) in enumerate(chunks):
                    mm(pn, pn[:, 0:nq], vp, pT[:, ci * nq:(ci + 1) * nq], ci == 0, ci == n - 1, vr + [pT])
                for ci in range(n):
                    mm(pd, pd[:, 0:nq], onesb[:, :], pT[:, ci * nq:(ci + 1) * nq], ci == 0, ci == n - 1, [onesb, pT])
                b = base
                if sink_i is not None:
                    kb.emit('dve', lambda e: e.tensor_scalar(rec[b:b + 64, 0:nq], pd[b:b + 64, 0:nq], sinkexp[b:b + 64, sink_i:sink_i + 1],
                                                             None, op0=ALU.add), reads=[pd, sinkexp], writes=[rec])
                    kb.emit('dve', lambda e: e.reciprocal(rec[b:b + 64, 0:nq], rec[b:b + 64, 0:nq]), reads=[rec], writes=[rec])
                else:
                    kb.emit('dve', lambda e: e.reciprocal(rec[b:b + 64, 0:nq], pd[b:b + 64, 0:nq]), reads=[pd], writes=[rec])
                kb.emit('dve', lambda e: e.tensor_tensor(out_ap, pn[b:b + 64, 0:nq], rec[b:b + 64, 0:nq], op=ALU.mult),
                        reads=[pn, rec], writes=[mixT])
            prev = ai.get('pend')
            ai['pend'] = phaseB
            if prev is not None:
                prev()

        def attn_flush():
            if ai.get('pend') is not None:
                ai['pend']()
                ai['pend'] = None

        for gi, (g0, gn, kind) in enumerate(GROUPS):
            cond = 0 if kind == 'p' else 1
            nch = gn // 128
            for ci in range(10):
                kb.dma(qk[:, ci, 0:gn], QKT.t[ci * 128:(ci + 1) * 128, g0:g0 + gn], reads=[QKT], writes=[qk])
            kb.dma(vv[:, 0:nch, :], VV.t[g0:g0 + gn, :].rearrange("(c p) n -> p c n", p=128), reads=[VV], writes=[vv])
            kb.dma(gg[:, 0:nch, :], GG.t[g0:g0 + gn, :].rearrange("(c p) n -> p c n", p=128), reads=[GG], writes=[gg])
            rowrep(gt1r, l, 16, cond)
            rowrep(sh2r, l, 24, cond)
            rowrep(gs2r, l, 32, cond, extra_g=gffn, plus1=True)
            if kind == 'p':
                for j in range(2):
                    pt = psum()
                    idx = 0
                    for sc in range(2):
                        for half in range(2):
                            mm(pt, pt[:, 0:256], gg[:, sc, half * 256 + j * 128:half * 256 + (j + 1) * 128],
                               dftp[:, sc, half * 256:(half + 1) * 256], idx == 0, idx == 3, [gg, dftp])
                            idx += 1
                    kb.emit('act', lambda e, p=pt, j=j: e.activation(mixT[:, j, 0:256], p[:, 0:256], AF.Copy),
                            reads=[pt], writes=[mixT])
            else:
                for st_ in range(16):
                    dc = dcb[st_ % 2]
                    ds_ = dsb[st_ % 2]
                    kb.dma(dc[:, :, :], dftc_d.t[:, st_ * 128:(st_ + 1) * 128].rearrange("(k p) n -> p k n", p=128),
                           reads=[dftc_d], writes=[dc])
                    kb.dma(ds_[:, :, :], dfts_d.t[:, st_ * 128:(st_ + 1) * 128].rearrange("(k p) n -> p k n", p=128),
                           reads=[dfts_d], writes=[ds_])
                    for j in range(2):
                        pt = psum()
                        for sc in range(16):
                            mm(pt, pt[:, 0:128], gg[:, sc, j * 128:(j + 1) * 128], dc[:, sc, :], sc == 0, False, [gg, dc])
                        for sc in range(16):
                            mm(pt, pt[:, 0:128], gg[:, sc, 256 + j * 128:256 + (j + 1) * 128], ds_[:, sc, :], False, sc == 15, [gg, ds_])
                        kb.emit('act', lambda e, p=pt, j=j, s0=st_ * 128: e.activation(mixT[:, j, s0:s0 + 128], p[:, 0:128], AF.Copy),
                                reads=[pt], writes=[mixT])
            if kind == 'p':
                for h in range(6):
                    base = 0 if h < 3 else 64
                    qc = h % 3
                    for qb in range(2):
                        chunks = []
                        for kc in range(2):
                            chunks.append((qk[base:base + 64, 3, kc * 128:(kc + 1) * 128], [qk],
                                           vv[:, kc, 0:128], [vv], None))
                        attn(lambda qc=qc, qb=qb, base=base: qk[base:base + 64, qc, qb * 128:(qb + 1) * 128], 128, chunks,
                             l * 6 + h, base, mixT[base:base + 64, 2 + qc, qb * 128:(qb + 1) * 128])
                for h in range(6):
                    base = (h % 2) * 64
                    pc = h // 2
                    for qb in range(2):
                        chunks = []
                        for kc in range(2):
                            chunks.append((qk[base:base + 64, 7 + pc, kc * 128:(kc + 1) * 128], [qk],
                                           vv[:, kc, 128 + pc * 128:128 + (pc + 1) * 128], [vv], None))
                        attn(lambda pc=pc, qb=qb, base=base: qk[base:base + 64, 4 + pc, qb * 128:(qb + 1) * 128], 128, chunks,
                             None, base, mixT[base:base + 64, 5 + pc, qb * 128:(qb + 1) * 128])
            else:
                for h in range(6):
                    base = 0 if h < 3 else 64
                    qc = h % 3
                    for qb in range(16):
                        chunks = []
                        for kc in (qb - 1, qb, qb + 1):
                            if kc < 0 or kc > 15:
                                continue
                            bias = None
                            if kc == qb - 1:
                                bias = (wmask, wmask[:, 0:128])
                            elif kc == qb + 1:
                                bias = (wmask, wmask[:, 128:256])
                            chunks.append((qk[base:base + 64, 3, kc * 128:(kc + 1) * 128], [qk],
                                           vv[:, kc, 0:128], [vv], bias))
                        for cc in range(2):
                            chunks.append((kcT[base:base + 64, 0, cc * 128:(cc + 1) * 128], [kcT],
                                           vc[:, cc, 0:128], [vc], None))
                        attn(lambda qc=qc, qb=qb, base=base: qk[base:base + 64, qc, qb * 128:(qb + 1) * 128], 128, chunks,
                             l * 6 + h, base, mixT[base:base + 64, 2 + qc, qb * 128:(qb + 1) * 128])
                bi = 0
                for jp in range(16):
                    case = {0: 0, 1: 1, 14: 3, 15: 4}.get(jp, 2)
                    R0 = min(max(2 * jp - 4, 0), 22)
                    kc0 = R0 // 2
                    for h in range(6):
                        base = (h % 2) * 64
                        pc = h // 2
                        nf = nbp[bi % 2]
                        nb = nbb[bi % 2]
                        bi += 1
                        kb.dma(nf[:, :], nbias_d.t[l, case, h, :, :], reads=[nbias_d], writes=[nf])
                        kb.emit('act', lambda e, nf=nf, nb=nb: e.activation(nb[:, :], nf[:, :], AF.Copy), reads=[nf], writes=[nb])
                        chunks = []
                        for i in range(5):
                            kc = kc0 + i
                            chunks.append((qk[base:base + 64, 7 + pc, kc * 128:(kc + 1) * 128], [qk],
                                           vv[:, kc, 128 + pc * 128:128 + (pc + 1) * 128], [vv], (nb, nb[:, i * 128:(i + 1) * 128])))
                        for cc in range(2):
                            chunks.append((kcT[base:base + 64, 1 + pc, cc * 128:(cc + 1) * 128], [kcT],
                                           vc[:, cc, 128 + pc * 128:128 + (pc + 1) * 128], [vc], None))
                        attn(lambda pc=pc, jp=jp, base=base: qk[base:base + 64, 4 + pc, jp * 128:(jp + 1) * 128], 128, chunks,
                             None, base, mixT[base:base + 64, 5 + pc, jp * 128:(jp + 1) * 128])
            attn_flush()
            if DBG:
                for k in range(8):
                    kb.dma(dbgMIX.t[l, k * 128:(k + 1) * 128, g0:g0 + gn], mixT[:, k, 0:gn], reads=[mixT], writes=[dbgMIX])
            for c in range(nch):
                ch = g0 // 128 + c
                xt = xtp[ch % 2]
                xn = xnp[ch % 2]
                kb.dma(xt[:, :], xsrc.t[ch * 128:(ch + 1) * 128, :], reads=[xsrc], writes=[xt])
                for hf in range(2):
                    pt = psum()
                    for k in range(8):
                        mm(pt, pt[:, :], mixT[:, k, c * 128:(c + 1) * 128], wob[:, k, hf * 512:(hf + 1) * 512], k == 0, k == 7, [mixT, wob])
                    kb.emit('dve', lambda e, p=pt, xn=xn, hf=hf: e.tensor_tensor(xn[:, hf * 512:(hf + 1) * 512], p[:, :],
                                                                                   gt1r[:, hf * 512:(hf + 1) * 512], op=ALU.mult),
                            reads=[pt, gt1r], writes=[xn])
                kb.emit('dve', lambda e, xt=xt, xn=xn: e.tensor_tensor(xt[:, :], xt[:, :], xn[:, :], op=ALU.add), reads=[xt, xn], writes=[xt])
                kb.dma(X.t[ch * 128:(ch + 1) * 128, :], xt[:, :], reads=[xt], writes=[X])
                if DBG:
                    kb.dma(dbgXT.t[l, ch * 128:(ch + 1) * 128, :], xt[:, :], reads=[xt], writes=[dbgXT])
                rmsnorm_chunk(xt, xn, ssqp[ch % 2], rstdp[ch % 2])
                h2 = h2p[ch % 2]
                h2b = h2bp[ch % 2]
                kb.emit('dve', lambda e, xn=xn, h2=h2: e.tensor_tensor(h2[:, :], xn[:, :], gs2r[:, :], op=ALU.mult), reads=[xn, gs2r], writes=[h2])
                kb.emit('dve', lambda e, h2=h2: e.tensor_tensor(h2[:, :], h2[:, :], sh2r[:, :], op=ALU.add), reads=[h2, sh2r], writes=[h2])
                kb.emit('act', lambda e, h2=h2, h2b=h2b: e.activation(h2b[:, :], h2[:, :], AF.Copy), reads=[h2], writes=[h2b])
                kb.dma(H2.t[ch * 128:(ch + 1) * 128, :], h2b[:, :], reads=[h2b], writes=[H2])
                h2T = h2Tp[ch % 2]
                for k in range(8):
                    pt = psum()
                    tr(pt, pt[:, 0:128], h2[:, k * 128:(k + 1) * 128], ident[:, :], [h2, ident])
                    kb.emit('act' if k % 2 else 'dve',
                            (lambda e, p=pt, h2T=h2T, k=k: e.activation(h2T[:, k, :], p[:, 0:128], AF.Copy)) if k % 2 else
                            (lambda e, p=pt, h2T=h2T, k=k: e.tensor_copy(h2T[:, k, :], p[:, 0:128])),
                            reads=[pt], writes=[h2T])
                pt = psum()
                for k in range(8):
                    mm(pt, pt[0:16, 0:128], wrt[:, k, :], h2T[:, k, :], k == 0, k == 7, [wrt, h2T])
                lg = lgp[ch % 2]
                kb.emit('act', lambda e, p=pt, lg=lg: e.activation(lg[:, :], p[0:16, 0:128], AF.Exp), reads=[pt], writes=[lg])
                p2 = psum()
                mm(p2, p2[0:16, 0:128], onesf[0:16, 0:16], lg[:, :], True, True, [onesf, lg])
                s1 = sm1[ch % 2]
                kb.emit('dve', lambda e, p2=p2, s1=s1: e.reciprocal(s1[:, :], p2[0:16, 0:128]), reads=[p2], writes=[s1])
                af = affp[ch % 2]
                kb.emit('dve', lambda e, lg=lg, s1=s1, af=af: e.tensor_tensor(af[:, :], lg[:, :], s1[:, :], op=ALU.mult),
                        reads=[lg, s1], writes=[af])
                kb.dma(AFFT.t[:, ch * 128:(ch + 1) * 128], af[:, :], reads=[af], writes=[AFFT])
                if DBG:
                    kb.dma(dbgAFF.t[l, :, ch * 128:(ch + 1) * 128], af[:, :], reads=[af], writes=[dbgAFF])
        kb.run()

        newps(ded=6)
        dgp = [kb.sb([128, 128], F32, "dg") for _ in range(2)]
        acc = kb.sb([128, 20, D], F32, "acc")
        for ch in range(20):
            kb.dma(acc[:, ch, :], X.t[ch * 128:(ch + 1) * 128, :], reads=[X], writes=[acc])
        gt2r = [kb.sb([128, D], F32, "gt2r") for _ in range(2)]
        rowrep(gt2r[0], l, 40, 0)
        rowrep(gt2r[1], l, 40, 1)
        arepL = [kb.sb([128, NT], F32, "arep")] * 2
        acolL = [kb.sb([128, 20], F32, "acol")] * 2

        def aload(ex_):
            b_ = ex_ % 2
            kb.dma(arepL[b_][:, :], AFFT.t[ex_:ex_ + 1, :].to_broadcast([128, NT]), reads=[AFFT], writes=[arepL[b_]])
            kb.dma(acolL[b_][:, :], AFFT.t[ex_, :].rearrange("(c p) -> p c", p=128), reads=[AFFT], writes=[acolL[b_]], slow=True)
        aload(0)
        rcol = kb.sb([128, 20], F32, "rcol")
        rrep = kb.sb([128, NT], F32, "rrep")
        junk = [kb.sb([128, 1024], F32, "junk") for _ in range(2)]
        nacol = kb.sb([128, 20], F32, "nacol")
        rcolA = kb.sb([128, 20], F32, "rcolA")
        rcolB = kb.sb([128, 20], F32, "rcolB")
        selp = [kb.sb([128, 256], BF16, "sel") for _ in range(3)]
        selT = kb.sb([128, 2, 2048], BF16, "selT")
        selP = kb.sb([128, 512], BF16, "selP")
        kb.emit('dve', lambda e: e.memset(selP[:, :], 0.0), writes=[selP])
        h2c = [kb.sb([128, D], BF16, "h2c") for _ in range(6)]
        xsT = kb.sb([128, 8, 320], BF16, "xsT")
        NST = 2
        wgb = [kb.sb([128, 8, 512], BF16, "wgb") for _ in range(NST)]
        wub = [kb.sb([128, 8, 512], BF16, "wub") for _ in range(NST)]
        wdb = [kb.sb([128, 4, D], BF16, "wdb") for _ in range(NST)]
        FT = [(0, 512), (512, 512), (1024, 512), (1536, 512), (2048, 512), (2560, 256)]
        NFT = len(FT)

        def wload(idx):
            ex_, ft_ = divmod(idx, NFT)
            s_ = idx % NST
            f0_, fw_ = FT[ft_]
            kb.dma(wgb[s_][:, :, 0:fw_], wg_d.t[l, ex_, :, f0_:f0_ + fw_].rearrange("(k p) n -> p k n", p=128), reads=[wg_d], writes=[wgb[s_]], q='pool')
            kb.dma(wub[s_][:, :, 0:fw_], wu_d.t[l, ex_, :, f0_:f0_ + fw_].rearrange("(k p) n -> p k n", p=128), reads=[wu_d], writes=[wub[s_]], q='pool')
            kb.dma(wdb[s_][:, 0:fw_ // 128, :], wd_d.t[l, ex_, f0_:f0_ + fw_, :].rearrange("(c p) n -> p c n", p=128), reads=[wd_d], writes=[wdb[s_]], q='pool')
        for i_ in range(NST - 1):
            wload(i_)
        sg = [kb.sb([128, 320], F32, "sg") for _ in range(2)]
        hm = [kb.sb([128, 320], BF16, "hm") for _ in range(2)]
        ysb = kb.sb([128, 3, D], BF16, "ysb")
        grp = [(512, 2048, 256, 0, 1), (0, 256, 32, 256, 0), (256, 256, 32, 288, 0)]
        jis = {'i': 0}

        def rank_prep(ex_):
            if ex_ > 0:
                aload(ex_)
            kb.emit('dve', lambda e: e.tensor_scalar(nacol[:, :], acolL[0][:, :], -1.0, None, op0=ALU.mult), reads=[acolL[0]], writes=[nacol])

        def rank_pieces():
            lst = []
            for (t0, n, cap, s0, cond) in grp:
                for c in range(n // 128):
                    ch = t0 // 128 + c
                    pieces = [(t0, 1024, rcolA), (t0 + 1024, 1024, rcolB)] if n == 2048 else [(t0, n, rcolA)]
                    for (p0, pn_, dst) in pieces:
                        def piece(ch=ch, p0=p0, pn_=pn_, dst=dst):
                            jk = junk[jis['i'] % 2]
                            jis['i'] += 1
                            kb.emit('act', lambda e, jk=jk: e.activation(
                                jk[:, 0:pn_], arepL[0][:, p0:p0 + pn_], AF.Sign, bias=nacol[:, ch:ch + 1]),
                                reads=[arepL[0], nacol], writes=[jk])
                            kb.emit('dve', lambda e, jk=jk: e.tensor_reduce(dst[:, ch:ch + 1], jk[:, 0:pn_], axis=AX.X, op=ALU.add),
                                    reads=[jk], writes=[dst])
                        lst.append(piece)
            return lst
        rank_prep(0)
        for pc_ in rank_pieces():
            pc_()
        ji = 0
        for ex in range(NE):
            arep = arepL[0]
            acol = acolL[0]
            kb.emit('dve', lambda e: e.tensor_tensor(rcol[:, 4:20], rcolA[:, 4:20], rcolB[:, 4:20], op=ALU.add), reads=[rcolA, rcolB], writes=[rcol])
            kb.emit('dve', lambda e: e.tensor_scalar(rcol[:, 4:20], rcol[:, 4:20], 0.5, 1023.5, op0=ALU.mult, op1=ALU.add), reads=[rcol], writes=[rcol])
            kb.emit('dve', lambda e: e.tensor_scalar(rcol[:, 0:4], rcolA[:, 0:4], 0.5, 127.5, op0=ALU.mult, op1=ALU.add), reads=[rcolA], writes=[rcol])
            if DBG:
                kb.dma(dbgR.t[l, ex, :, :], rcol[:, :], reads=[rcol], writes=[dbgR])
            kb.dma(RNKT.t[ex, :].rearrange("(c p) -> p c", p=128), rcol[:, :], reads=[rcol], writes=[RNKT], slow=True)
            si = 0
            for (t0, n, cap, s0, cond) in grp:
                nchg = n // 128
                pts = PS[0:4] if cap == 256 else [PS[4]]
                for c in range(nchg):
                    ch = t0 // 128 + c
                    sel = selp[si % 3]
                    hc = h2c[si % 6]
                    si += 1
                    kb.dma(hc[:, :], H2.t[ch * 128:(ch + 1) * 128, :], reads=[H2], writes=[hc])
                    kb.emit('dve', lambda e, sel=sel, ch=ch, cap=cap: e.tensor_scalar(
                        sel[:, 0:cap], iotar[:, 0:cap], rcol[:, ch:ch + 1], None, op0=ALU.is_equal),
                        reads=[iotar, rcol], writes=[sel])
                    for k in range(8):
                        if cap == 256:
                            pt = pts[k // 2]
                            o = (k % 2) * 256
                        else:
                            pt = pts[0]
                            o = k * 32
                        first = (c == 0) and ((k % 2 == 0) if cap == 256 else (k == 0))
                        mm(pt, pt[:, o:o + cap], hc[:, k * 128:(k + 1) * 128], sel[:, 0:cap], first, c == nchg - 1, [hc, sel], skip=True)
                for k in range(8):
                    if cap == 256:
                        pt = pts[k // 2]
                        o = (k % 2) * 256
                    else:
                        pt = pts[0]
                        o = k * 32
                    use_act = ((k // 2) % 2 == 1) if cap == 256 else True
                    kb.emit('act' if use_act else 'dve',
                            (lambda e, p=pt, o=o, k=k, s0=s0, cap=cap: e.activation(xsT[:, k, s0:s0 + cap], p[:, o:o + cap], AF.Copy)) if use_act else
                            (lambda e, p=pt, o=o, k=k, s0=s0, cap=cap: e.tensor_copy(xsT[:, k, s0:s0 + cap], p[:, o:o + cap])),
                            reads=[pt], writes=[xsT])
            kb.dma(rrep[:, :], RNKT.t[ex:ex + 1, :].to_broadcast([128, NT]), reads=[RNKT], writes=[rrep])
            kb.emit('dve', lambda e, arep=arep: e.scalar_tensor_tensor(selT[:, 0, :], rrep[:, 512:2560], iotac[:, 0:1], arep[:, 512:2560],
                                                            op0=ALU.is_equal, op1=ALU.mult), reads=[rrep, iotac, arep], writes=[selT])
            kb.emit('dve', lambda e, arep=arep: e.scalar_tensor_tensor(selT[:, 1, :], rrep[:, 512:2560], iotac[:, 1:2], arep[:, 512:2560],
                                                             op0=ALU.is_equal, op1=ALU.mult), reads=[rrep, iotac, arep], writes=[selT])
            kb.emit('dve', lambda e, arep=arep: e.scalar_tensor_tensor(selP[0:32, 0:256], rrep[0:32, 0:256], iotac[0:32, 0:1], arep[0:32, 0:256],
                                                            op0=ALU.is_equal, op1=ALU.mult), reads=[rrep, iotac, arep], writes=[selP])
            kb.emit('dve', lambda e, arep=arep: e.scalar_tensor_tensor(selP[32:64, 256:512], rrep[32:64, 256:512], iotac[32:64, 2:3], arep[32:64, 256:512],
                                                            op0=ALU.is_equal, op1=ALU.mult), reads=[rrep, iotac, arep], writes=[selP])
            nxt = []
            if ex + 1 < NE:
                rank_prep(ex + 1)
                nxt = rank_pieces()
            py = PS[0:4]
            ypr = PS[4:6]
            pend = None

            def down(fc_, b_, ws_, j_):
                for sc in range(2):
                    for hf in range(2):
                        pt = py[sc * 2 + hf]
                        mm(pt, pt[:, :], hm[b_][:, sc * 128:(sc + 1) * 128], wdb[ws_][:, j_, hf * 512:(hf + 1) * 512], fc_ == 0, fc_ == NFC - 1, [hm[b_], wdb[ws_]])
                for hf in range(2):
                    mm(ypr[hf], ypr[hf][0:64, :], hm[b_][:, 256:320],
                       wdb[ws_][:, j_, hf * 512:(hf + 1) * 512], fc_ == 0, fc_ == NFC - 1, [hm[b_], wdb[ws_]])
            for ft in range(NFT):
                widx = ex * NFT + ft
                ws = widx % NST
                for j in range(FT[ft][1] // 128):
                    fc = FT[ft][0] // 128 + j
                    b = fc % 2
                    pa = psum()
                    pu = psum()
                    for k in range(8):
                        mm(pa, pa[:, 0:320], wgb[ws][:, k, j * 128:(j + 1) * 128], xsT[:, k, :], k == 0, k == 7, [wgb[ws], xsT])
                    for k in range(8):
                        mm(pu, pu[:, 0:320], wub[ws][:, k, j * 128:(j + 1) * 128], xsT[:, k, :], k == 0, k == 7, [wub[ws], xsT])
                    if pend is not None:
                        down(*pend)
                        pend = None
                    if j == 0 and widx + NST - 1 < NE * NFT:
                        wload(widx + NST - 1)
                    kb.emit('act', lambda e, pa=pa, b=b: e.activation(sg[b][:, :], pa[:, 0:320], AF.Silu), reads=[pa], writes=[sg[b]])
                    kb.emit('dve', lambda e, pu=pu, b=b: e.tensor_tensor(hm[b][:, :], pu[:, 0:320], sg[b][:, :], op=ALU.mult),
                            reads=[pu, sg[b]], writes=[hm[b]])
                    for _ in range(2):
                        if nxt:
                            nxt.pop(0)()
                    pend = (fc, b, ws, j)
            down(*pend)
            while nxt:
                nxt.pop(0)()
            for sc in range(2):
                for hf in range(2):
                    pt = py[sc * 2 + hf]
                    kb.emit('dve', lambda e, p=pt, sc=sc, hf=hf: e.tensor_tensor(ysb[:, sc, hf * 512:(hf + 1) * 512], p[:, :],
                                                                                  gt2r[1][:, hf * 512:(hf + 1) * 512], op=ALU.mult),
                            reads=[pt, gt2r[1]], writes=[ysb])
            for hf in range(2):
                kb.emit('dve', lambda e, hf=hf, p=ypr[hf]: e.tensor_tensor(ysb[0:64, 2, hf * 512:(hf + 1) * 512], p[0:64, :],
                                                                             gt2r[0][0:64, hf * 512:(hf + 1) * 512], op=ALU.mult),
                        reads=[ypr[hf], gt2r[0]], writes=[ysb])
            for ch in range(20):
                sample = ch >= 4
                cond = 1 if sample else 0
                for hf in range(2):
                    pt = PS[(ch * 2 + hf) % 8]
                    if sample:
                        for sc in range(2):
                            mm(pt, pt[:, :], selT[:, sc, (ch - 4) * 128:(ch - 3) * 128], ysb[:, sc, hf * 512:(hf + 1) * 512], sc == 0, sc == 1, [selT, ysb])
                    else:
                        mm(pt, pt[:, :], selP[0:64, ch * 128:(ch + 1) * 128], ysb[0:64, 2, hf * 512:(hf + 1) * 512], True, True, [selP, ysb])
                    kb.emit('dve', lambda e, p=pt, ch=ch, hf=hf: e.tensor_tensor(acc[:, ch, hf * 512:(hf + 1) * 512], p[:, :],
                                                                                  acc[:, ch, hf * 512:(hf + 1) * 512], op=ALU.add),
                            reads=[pt, acc], writes=[acc])
        if DBG:
            for ch in range(20):
                kb.dma(dbgXF.t[l, ch * 128:(ch + 1) * 128, :], acc[:, ch, :], reads=[acc], writes=[dbgXF])
        if l < DEPTH - 1:
            for ch in range(20):
                kb.dma(X.t[ch * 128:(ch + 1) * 128, :], acc[:, ch, :], reads=[acc], writes=[X])
        else:
            gfin = gt2r[0]
            kb.dma(gfin[:, :], gfin_d[:, :], reads=[gfin_d], writes=[gfin])
            xnp = [arepL[0], rrep]
            ssqp = [kb.sb([128, 1], F32, "ssq") for _ in range(2)]
            rstdp = [kb.sb([128, 1], F32, "rstd") for _ in range(2)]
            for ch in range(20):
                xn = xnp[ch % 2]
                ssq = ssqp[ch % 2]
                rstd = rstdp[ch % 2]
                kb.emit('act', lambda e, xn=xn, ch=ch: e.activation(xn[:, 0:D], acc[:, ch, :], AF.Square), reads=[acc], writes=[xn])
                kb.emit('dve', lambda e, xn=xn, ssq=ssq: e.tensor_reduce(ssq[:, 0:1], xn[:, 0:D], axis=AX.X, op=ALU.add), reads=[xn], writes=[ssq])
                kb.emit('dve', lambda e, ssq=ssq, rstd=rstd: e.tensor_scalar(rstd[:, 0:1], ssq[:, 0:1], 1.0 / D, 1e-6, op0=ALU.mult, op1=ALU.add),
                        reads=[ssq], writes=[rstd])
                kb.emit('act', lambda e, rstd=rstd: e.activation(rstd[:, 0:1], rstd[:, 0:1], AF.Sqrt), reads=[rstd], writes=[rstd])
                kb.emit('dve', lambda e, rstd=rstd: e.reciprocal(rstd[:, 0:1], rstd[:, 0:1]), reads=[rstd], writes=[rstd])
                kb.emit('dve', lambda e, xn=xn, rstd=rstd, ch=ch: e.scalar_tensor_tensor(xn[:, 0:D], acc[:, ch, :], rstd[:, 0:1], gfin[:, :],
                                                                                        op0=ALU.mult, op1=ALU.mult), reads=[acc, rstd, gfin], writes=[xn])
                kb.dma(yout.t[ch * 128:(ch + 1) * 128, :], xn[:, 0:D], reads=[xn], writes=[yout])
        kb.run()
    es.close()
    return nc


_NC = None


def _consts():
    c = {}
    c["ident"] = np.eye(128, dtype=np.float32)
    c["identb"] = np.eye(128, dtype=np.float32).astype(NPBF)
    P = np.zeros((128, 128), np.float32)
    for m in range(128):
        if m % 32 < 16:
            P[m, m + 16] = -1.0
        else:
            P[m, m - 16] = 1.0
    c["prot"] = np.ascontiguousarray(P.T)
    pos = np.arange(2048)
    inv = 1.0 / (10000.0 ** (np.arange(16, dtype=np.float32) / 16))
    dd = np.arange(128) % 64
    half = dd // 32
    j = dd % 16
    coord = np.where(half[:, None] == 0, (pos // 64)[None, :], (pos % 64)[None, :]).astype(np.float32)
    ang = coord * inv[j][:, None].astype(np.float32)
    c["ropecos"] = np.cos(ang).astype(np.float32)
    c["ropesin"] = np.sin(ang).astype(np.float32)
    kk = np.arange(128)[:, None]
    qq = np.arange(128)[None, :]
    prev = np.where(qq <= kk, 0.0, NEG)
    nxt = np.where(kk <= qq, 0.0, NEG)
    c["wmask"] = np.concatenate([prev, nxt], axis=1).astype(np.float32).astype(NPBF)
    s2 = np.arange(2048, dtype=np.float64)
    a = 2 * np.pi * np.outer(s2, s2) / 2048.0
    sc = 1.0 / np.sqrt(2048.0 * 64.0)
    c["dftc"] = (np.cos(a) * sc).astype(np.float32).astype(NPBF)
    c["dfts"] = (-np.sin(a) * sc).astype(np.float32).astype(NPBF)
    s1 = np.arange(256, dtype=np.float64)
    a = 2 * np.pi * np.outer(s1, s1) / 256.0
    sc = 1.0 / np.sqrt(256.0 * 64.0)
    c["dftp"] = np.concatenate([np.cos(a) * sc, -np.sin(a) * sc], axis=1).astype(np.float32).astype(NPBF)
    cc = np.arange(256)
    same = (cc[:, None] // 64) == (cc[None, :] // 64)
    b = 2 * np.pi * np.outer(cc % 64, cc % 64) / 64.0
    c["cdsd"] = np.concatenate([np.where(same, np.cos(b), 0.0), np.where(same, np.sin(b), 0.0)], axis=1).astype(np.float32).astype(NPBF)
    c["iota_row"] = np.tile(np.arange(512, dtype=np.float32)[None, :], (128, 1))
    p = np.arange(128, dtype=np.float32)
    c["iota_col"] = np.stack([p, p + 128, p - 32, p], axis=1).astype(np.float32)
    oh = np.zeros((16, 16, 128), np.float32)
    for e in range(16):
        oh[e, e, :] = 1.0
    c["onehot16"] = oh.reshape(16, 16 * 128)
    return c


def _nbias(rpb):
    out = np.full((DEPTH, 5, 6, 128, 5, 128), NEG, np.float32)
    for case, jp in enumerate([0, 1, 5, 14, 15]):
        R0 = min(max(2 * jp - 4, 0), 22)
        q = 128 * jp + np.arange(128)
        rq = q // 64
        cq = q % 64
        rs = np.clip(rq - 4, 0, 24)
        cs = np.clip(cq - 8, 0, 48)
        for i in range(5):
            k = 128 * (R0 // 2 + i) + np.arange(128)
            rk = k // 64
            ck = k % 64
            ok = (rk[:, None] >= rs[None, :]) & (rk[:, None] < rs[None, :] + 8) & \
                 (ck[:, None] >= cs[None, :]) & (ck[:, None] < cs[None, :] + 16)
            rr = np.clip(rk[:, None] - rq[None, :] + 7, 0, 14)
            rc = np.clip(ck[:, None] - cq[None, :] + 15, 0, 30)
            vals = rpb[:, :, rr, rc]
            out[:, case, :, :, i, :] = np.where(ok[None, None], vals, NEG)
    return out.reshape(DEPTH, 5, 6, 128, 640)


def kernel(x_prompt, x_sample, cache_win_k, cache_win_v, cache_nat_k, cache_nat_v, c, c_ctx, w_mod, b_mod,
           g_mix, g_ffn, w_in, w_out, win_sink, nat_rpb, w_router, w_gate, w_up, w_down, g_final):
    global _NC
    f = lambda a: np.ascontiguousarray(np.asarray(a, dtype=np.float32))
    x_prompt, x_sample = f(x_prompt), f(x_sample)
    if _NC is None:
        _NC = build()
    nc = _NC
    cst = _consts()
    shared = dict(cst)
    shared["w_mod"] = f(w_mod)
    shared["bmodT"] = f(np.asarray(b_mod).reshape(2, 48, 128).transpose(2, 0, 1).reshape(128, 96))
    shared["gmixT"] = f(np.asarray(g_mix).reshape(2, 8, 128).transpose(2, 0, 1).reshape(128, 16))
    shared["gffn_rep"] = f(np.tile(np.asarray(g_ffn).reshape(1, 2 * D), (128, 1)))
    shared["gfin_rep"] = f(np.tile(np.asarray(g_final).reshape(1, D), (128, 1)))
    shared["w_in"] = f(w_in)
    shared["w_out"] = f(w_out)
    shared["w_router"] = f(w_router)
    shared["w_gate"] = f(w_gate)
    shared["w_up"] = f(w_up)
    shared["w_down"] = f(w_down)
    shared["sink_rep"] = f(np.tile(np.asarray(win_sink).reshape(1, 12), (128, 1)))
    shared["nbias"] = _nbias(np.asarray(nat_rpb, dtype=np.float32))
    in_maps = []
    for core in range(8):
        sq = core // 4
        m = dict(shared)
        m["xin"] = np.ascontiguousarray(np.concatenate([x_prompt[2 * core], x_prompt[2 * core + 1], x_sample[sq]], axis=0))
        cond = np.stack([np.asarray(c_ctx, dtype=np.float32), np.asarray(c, dtype=np.float32)[sq]], axis=0)
        m["condT"] = f(cond.reshape(2, 8, 128).transpose(2, 0, 1).reshape(128, 16))
        m["ckw"] = f(np.asarray(cache_win_k)[sq].reshape(2, 256, 128))
        m["cvw"] = f(np.asarray(cache_win_v)[sq].reshape(2, 256, 128))
        m["ckn"] = f(np.asarray(cache_nat_k)[sq].reshape(2, 256, 384))
        m["cvn"] = f(np.asarray(cache_nat_v)[sq].reshape(2, 256, 384))
        in_maps.append(m)
    res = run_bass_kernel_spmd(nc, in_maps, core_ids=list(range(8)))
    r = res.results
    y_prompt = np.zeros((16, 256, D), np.float32)
    y_sample = np.zeros((2, 2048, D), np.float32)
    nwk = np.zeros((16, 2, 256, 2, 64), np.float32)
    nwv = np.zeros((16, 2, 256, 2, 64), np.float32)
    nnk = np.zeros((16, 2, 256, 6, 64), np.float32)
    nnv = np.zeros((16, 2, 256, 6, 64), np.float32)
    for core in range(8):
        yo = np.asarray(r[core]["yout"])
        y_prompt[2 * core] = yo[0:256]
        y_prompt[2 * core + 1] = yo[256:512]
        q = core % 4
        y_sample[core // 4, q * 512:(q + 1) * 512] = yo[512 + q * 512:512 + (q + 1) * 512]
        for j in range(2):
            nwk[2 * core + j] = np.asarray(r[core]["kwout"])[j].reshape(2, 256, 2, 64)
            nwv[2 * core + j] = np.asarray(r[core]["vwout"])[j].reshape(2, 256, 2, 64)
            nnk[2 * core + j] = np.asarray(r[core]["knout"])[j].reshape(2, 256, 6, 64)
            nnv[2 * core + j] = np.asarray(r[core]["vnout"])[j].reshape(2, 256, 6, 64)
    global _DBG
    _DBG = r
    return (y_prompt, y_sample, nwk, nwv, nnk, nnv)
```
